# Optimizing a Trainium2 kernel written in Bass

```python
import math
import jax, jax.numpy as jnp
from jax import lax
import numpy as np

D_MODEL = 2048
BATCH = 4
SEQ = 4096
DEPTH = 1

CHUNK = 64
N_META = 16
D_MIX = D_MODEL
FOX_HEADS = 8
FOX_HEAD_DIM = 128
D_FOX = FOX_HEADS * FOX_HEAD_DIM
POOL_WINDOWS = (2, 4, 8, 16)
N_POOL_GROUPS = len(POOL_WINDOWS)
D_POOL = D_MIX - D_FOX
POOL_GROUP_DIM = D_POOL // N_POOL_GROUPS
D_IN_PROJ = 3 * D_FOX + FOX_HEADS + D_POOL
Q_BLOCK = 128
N_GROUPS = 4
EXPERTS_PER_GROUP = 8
N_EXPERTS = N_GROUPS * EXPERTS_PER_GROUP
TOP_K = 2
D_EXPERT = 1024
ROW_BLOCK = 128
LN_EPS = 1e-5
DEEPNORM_ALPHA = (2.0 * DEPTH) ** 0.25
DEEPNORM_BETA = (8.0 * DEPTH) ** -0.25

kernel_name = "fox_pool_hier_moe_deepnorm_trunk"


def layer_norm(x, g, b):
    xf = x.astype(jnp.float32)
    mu = jnp.mean(xf, axis=-1, keepdims=True)
    var = jnp.mean(jnp.square(xf - mu), axis=-1, keepdims=True)
    return ((xf - mu) * lax.rsqrt(var + LN_EPS) * g + b).astype(x.dtype)


def forgetting_attention(q, k, v, log_f):
    B_, H, L, hd = q.shape
    Lp = ((L + Q_BLOCK - 1) // Q_BLOCK) * Q_BLOCK
    pad = Lp - L
    q = jnp.pad(q, ((0, 0), (0, 0), (0, pad), (0, 0)))
    k = jnp.pad(k, ((0, 0), (0, 0), (0, pad), (0, 0)))
    v = jnp.pad(v, ((0, 0), (0, 0), (0, pad), (0, 0)))
    c = jnp.cumsum(jnp.pad(log_f, ((0, 0), (0, 0), (0, pad))), axis=-1)
    nq = Lp // Q_BLOCK
    q_blocks = q.reshape(B_, H, nq, Q_BLOCK, hd).transpose(2, 0, 1, 3, 4)
    c_blocks = c.reshape(B_, H, nq, Q_BLOCK).transpose(2, 0, 1, 3)
    starts = jnp.arange(nq, dtype=jnp.int32) * Q_BLOCK
    key_pos = jnp.arange(Lp, dtype=jnp.int32)
    scale = hd ** -0.5

    def query_block(args):
        q_i, c_i, start = args
        s = jnp.einsum('bhqd,bhkd->bhqk', q_i, k,
                       preferred_element_type=jnp.float32) * scale
        s = s + c_i[..., :, None] - c[:, :, None, :]
        causal = (start + jnp.arange(Q_BLOCK, dtype=jnp.int32))[:, None] >= key_pos[None, :]
        s = jnp.where(causal, s, -jnp.inf)
        p = jax.nn.softmax(s, axis=-1)
        return jnp.einsum('bhqk,bhkd->bhqd', p.astype(v.dtype), v)

    o = lax.map(query_block, (q_blocks, c_blocks, starts))
    o = o.transpose(1, 0, 3, 2, 4).reshape(B_, Lp, H * hd)
    return o[:, :L]


def multiscale_pool(u, pool_w, pool_scale):
    B_, L, _ = u.shape
    uf = u.astype(jnp.float32).reshape(B_, L, N_POOL_GROUPS, POOL_GROUP_DIM)
    cs = jnp.cumsum(uf, axis=1)
    t = jnp.arange(L)
    outs = []
    for g, w in enumerate(POOL_WINDOWS):
        cs_g = cs[:, :, g]
        prev = jnp.pad(cs_g, ((0, 0), (w, 0), (0, 0)))[:, :L]
        count = jnp.minimum(t + 1, w).astype(jnp.float32)[None, :, None]
        outs.append((cs_g - prev) / count - uf[:, :, g])
    pooled = jnp.stack(outs, axis=2).astype(u.dtype)
    mixed = jnp.einsum('blgc,gcd->blgd', pooled, pool_w)
    return mixed.reshape(B_, L, D_POOL) * pool_scale


def hybrid_mixer(h, w_in, b_f, pool_w, pool_scale, w_out):
    B_, L, _ = h.shape
    proj = h @ w_in
    q, k, v, f_logit, u = jnp.split(
        proj, [D_FOX, 2 * D_FOX, 3 * D_FOX, 3 * D_FOX + FOX_HEADS], axis=-1)

    def to_heads(t):
        return t.reshape(B_, L, FOX_HEADS, FOX_HEAD_DIM).transpose(0, 2, 1, 3)

    log_f = jax.nn.log_sigmoid((f_logit + b_f).astype(jnp.float32)).transpose(0, 2, 1)
    o_fox = forgetting_attention(to_heads(q), to_heads(k), to_heads(v), log_f)
    o_pool = multiscale_pool(u, pool_w, pool_scale)
    return jnp.concatenate([o_fox.astype(h.dtype), o_pool], axis=-1) @ w_out


def hierarchical_moe(h, w_rg, b_rg, w_re, b_re, w13, w2):
    B_, L, D = h.shape
    T = B_ * L
    xt = h.reshape(T, D)
    logits_g = (xt @ w_rg).astype(jnp.float32) + b_rg
    g_idx = jnp.argmax(logits_g, axis=-1)
    p_g = jnp.take_along_axis(jax.nn.softmax(logits_g, axis=-1), g_idx[:, None], axis=1)[:, 0]
    logits_e = ((xt @ w_re).astype(jnp.float32) + b_re).reshape(T, N_GROUPS, EXPERTS_PER_GROUP)
    le = jnp.take_along_axis(logits_e, g_idx[:, None, None], axis=1)[:, 0]
    top_v, top_i = lax.top_k(le, TOP_K)
    gates = (jax.nn.softmax(top_v, axis=-1) * p_g[:, None]).astype(xt.dtype)
    expert = (g_idx[:, None] * EXPERTS_PER_GROUP + top_i).astype(jnp.int32)

    A = T * TOP_K
    flat_e = expert.reshape(A)
    order = jnp.argsort(flat_e)
    e_sorted = flat_e[order]
    tok_sorted = (order // TOP_K).astype(jnp.int32)
    counts = jnp.bincount(flat_e, length=N_EXPERTS).astype(jnp.int32)
    start = jnp.cumsum(counts) - counts
    padded = ((counts + ROW_BLOCK - 1) // ROW_BLOCK) * ROW_BLOCK
    pend = jnp.cumsum(padded)
    pstart = pend - padded
    rank = jnp.arange(A, dtype=jnp.int32) - start[e_sorted]
    dest_sorted = pstart[e_sorted] + rank
    n_blocks = (A + N_EXPERTS * (ROW_BLOCK - 1) + ROW_BLOCK - 1) // ROW_BLOCK
    rows_tok = jnp.full((n_blocks * ROW_BLOCK,), T, jnp.int32).at[dest_sorted].set(tok_sorted)
    block_expert = jnp.minimum(
        jnp.searchsorted(pend, jnp.arange(n_blocks, dtype=jnp.int32) * ROW_BLOCK, side='right'),
        N_EXPERTS - 1).astype(jnp.int32)
    x_ext = jnp.concatenate([xt, jnp.zeros((1, D), xt.dtype)], axis=0)

    def expert_block(args):
        tok_idx, e = args
        xb = x_ext[tok_idx]
        gate, up = jnp.split(xb @ w13[e], 2, axis=-1)
        return (jax.nn.silu(gate) * up) @ w2[e]

    out = lax.map(expert_block, (rows_tok.reshape(n_blocks, ROW_BLOCK), block_expert))
    out = out.reshape(n_blocks * ROW_BLOCK, D)
    dest = jnp.zeros((A,), jnp.int32).at[order].set(dest_sorted)
    y = jnp.sum(out[dest].reshape(T, TOP_K, D) * gates[..., None], axis=1)
    return y.reshape(B_, L, D)


def setup_inputs(seed: int = 0) -> dict:
    key = jax.random.key(seed)
    ks = jax.random.split(key, 20)
    f32 = jnp.float32
    nrm = lambda k, s: jax.random.normal(k, s, f32)
    d = D_MODEL
    x = nrm(ks[0], (BATCH, SEQ, d))
    meta = nrm(ks[1], (N_META, d))
    ln_in_g = 1.0 + 0.02 * nrm(ks[2], (d,))
    ln_in_b = 0.02 * nrm(ks[3], (d,))
    w_in = nrm(ks[4], (DEPTH, d, D_IN_PROJ)) * d ** -0.5
    b_f = jnp.linspace(1.0, 6.0, FOX_HEADS, dtype=f32)[None, :] + 0.1 * nrm(ks[5], (DEPTH, FOX_HEADS))
    pool_w = nrm(ks[6], (DEPTH, N_POOL_GROUPS, POOL_GROUP_DIM, POOL_GROUP_DIM)) * POOL_GROUP_DIM ** -0.5
    pool_scale = 1.0 + 0.02 * nrm(ks[7], (DEPTH, D_POOL))
    w_out = nrm(ks[8], (DEPTH, D_MIX, d)) * (D_MIX ** -0.5 * DEEPNORM_BETA)
    ln_mix_g = 1.0 + 0.02 * nrm(ks[9], (DEPTH, d))
    ln_mix_b = 0.02 * nrm(ks[10], (DEPTH, d))
    w_router_g = nrm(ks[11], (DEPTH, d, N_GROUPS)) * d ** -0.5
    b_router_g = 0.01 * nrm(ks[12], (DEPTH, N_GROUPS))
    w_router_e = nrm(ks[13], (DEPTH, d, N_EXPERTS)) * d ** -0.5
    b_router_e = 0.01 * nrm(ks[14], (DEPTH, N_EXPERTS))
    w13 = nrm(ks[15], (DEPTH, N_EXPERTS, d, 2 * D_EXPERT)) * d ** -0.5
    w2 = nrm(ks[16], (DEPTH, N_EXPERTS, D_EXPERT, d)) * (D_EXPERT ** -0.5 * DEEPNORM_BETA)
    ln_ffn_g = 1.0 + 0.02 * nrm(ks[17], (DEPTH, d))
    ln_ffn_b = 0.02 * nrm(ks[18], (DEPTH, d))
    return {"x": x, "meta": meta, "ln_in_g": ln_in_g, "ln_in_b": ln_in_b,
            "w_in": w_in, "b_f": b_f, "pool_w": pool_w, "pool_scale": pool_scale,
            "w_out": w_out, "ln_mix_g": ln_mix_g, "ln_mix_b": ln_mix_b,
            "w_router_g": w_router_g, "b_router_g": b_router_g,
            "w_router_e": w_router_e, "b_router_e": b_router_e,
            "w13": w13, "w2": w2, "ln_ffn_g": ln_ffn_g, "ln_ffn_b": ln_ffn_b}


def reference(x, meta, ln_in_g, ln_in_b, w_in, b_f, pool_w, pool_scale, w_out,
              ln_mix_g, ln_mix_b, w_router_g, b_router_g, w_router_e, b_router_e,
              w13, w2, ln_ffn_g, ln_ffn_b):
    B_ = x.shape[0]
    meta_b = jnp.broadcast_to(meta[None].astype(x.dtype), (B_, N_META, D_MODEL))
    h = jnp.concatenate([meta_b, x], axis=1)
    h = layer_norm(h, ln_in_g, ln_in_b)
    for l in range(DEPTH):
        mix = hybrid_mixer(h, w_in[l], b_f[l], pool_w[l], pool_scale[l], w_out[l])
        h = layer_norm(DEEPNORM_ALPHA * h + mix, ln_mix_g[l], ln_mix_b[l])
        ffn = hierarchical_moe(h, w_router_g[l], b_router_g[l], w_router_e[l], b_router_e[l],
                               w13[l], w2[l])
        h = layer_norm(DEEPNORM_ALPHA * h + ffn, ln_ffn_g[l], ln_ffn_b[l])
    return h[:, N_META:]
```

```python
import contextlib
import numpy as np
import concourse.bass as bass
import concourse.mybir as mybir
from concourse.bass_utils import run_bass_kernel_spmd

F32 = mybir.dt.float32
BF16 = mybir.dt.bfloat16
I32 = mybir.dt.int32
AF = mybir.ActivationFunctionType
ALU = mybir.AluOpType
AX = mybir.AxisListType

D = 2048
NCH = 33
LP = NCH * 128
NOWN = 2048
DIN = 4104
ALPHA = 2.0 ** 0.25
EPS = 1e-5
SCALE = 128.0 ** -0.5
NBLK = 64
TILES = {0: [0, 3, 4, 7], 1: [1, 2, 5, 6]}


class Sched:
    def __init__(self, nc, es):
        self.nc = nc
        self.eng = {'pe': nc.tensor, 'dve': nc.vector, 'act': nc.scalar,
                    'pool': nc.gpsimd, 'sp': nc.sync}
        self.sem = {k: es.enter_context(nc.semaphore("s_" + k)) for k in self.eng}
        self.seq = {k: 0 for k in self.eng}
        self.known = {k: {} for k in self.eng}
        self.lastw = {}
        self.readers = {}
        self.dsem = {}
        self.dcons = {}
        self.es = es
        self.muted = False

    def _dma_sem(self, name):
        if name not in self.dsem:
            self.dsem[name] = [self.es.enter_context(self.nc.semaphore("d_" + name)), 0]
        return self.dsem[name]

    def _wait(self, e, tok):
        kind, name, val = tok
        if kind == 'eng' and name == e and e in ('pe', 'sp'):
            return
        k = (kind, name)
        if self.known[e].get(k, 0) >= val:
            return
        self.known[e][k] = val
        sem = self.sem[name] if kind == 'eng' else self.dsem[name][0]
        if kind == 'dma':
            self.dcons[name] = max(self.dcons.get(name, 0), val)
        self.eng[e].wait_ge(sem, val)

    def deps(self, e, reads, writes):
        toks = []
        for r in reads:
            if r in self.lastw:
                toks.append(self.lastw[r])
        for w in writes:
            if w in self.lastw:
                toks.append(self.lastw[w])
            toks.extend(self.readers.get(w, ()))
        for t in toks:
            self._wait(e, t)

    def commit(self, tok, reads, writes):
        for r in reads:
            self.readers.setdefault(r, []).append(tok)
        for w in writes:
            self.lastw[w] = tok
            self.readers[w] = []

    def op(self, e, fn, reads=(), writes=()):
        if self.muted:
            return None
        self.deps(e, reads, writes)
        ins = fn()
        self.seq[e] += 1
        ins.then_inc(self.sem[e], 1)
        tok = ('eng', e, self.seq[e])
        self.commit(tok, reads, writes)
        return tok

    def dma(self, e, fn, sem, reads=(), writes=()):
        if self.muted:
            return None
        self.deps(e, reads, writes)
        s = self._dma_sem(sem)
        if self.dcons.get(sem, 0) > 0:
            self._wait(e, ('dma', sem, self.dcons[sem]))
        ins = fn()
        s[1] += 16
        ins.then_inc(s[0], 16)
        tok = ('dma', sem, s[1])
        self.commit(tok, reads, writes)
        return tok

    def regroup(self, tok, keys):
        if self.muted:
            return
        for k in keys:
            self.lastw[k] = tok

    def wait_all(self, e):
        for name, (s, c) in self.dsem.items():
            if c:
                self._wait(e, ('dma', name, c))
        for k in self.eng:
            if self.seq[k] and k != e:
                self._wait(e, ('eng', k, self.seq[k]))

    def barrier(self):
        for e in self.eng:
            self.wait_all(e)


def build(stop=99):
    DEBUG = stop < 99
    nc = bass.Bass("TRN2", target_bir_lowering=False)

    def din(name, shape, dt=F32):
        return nc.dram_tensor(name, shape, dt, kind="ExternalInput").ap()

    def dscr(name, shape, dt):
        return nc.dram_tensor(name, shape, dt, kind="ExternalOutput" if DEBUG else "Internal").ap()

    xfull = din("xfull", [LP, D])
    xown = din("xown", [4 * 640, D])
    w_in = din("w_in", [D, DIN])
    w_out = din("w_out", [D, D])
    pool_w = din("pool_w", [4 * 256, 256])
    w13 = din("w13", [32 * 2048 if stop >= 7 else 128, 2048])
    w2 = din("w2", [32 * 1024 if stop >= 7 else 128, 2048])
    dbg = nc.dram_tensor("dbg", [128, 4096], F32, kind="ExternalOutput").ap() if DEBUG else None
    dbgb = nc.dram_tensor("dbgb", [128, 16 * 640], BF16, kind="ExternalOutput").ap() if DEBUG else None
    gT = din("gT", [128, 16])
    bT = din("bT", [128, 16])
    gb_d = din("gb", [128, 6 * D])
    pscale = din("pscale", [128, 8])
    bf_b = din("bf_b", [128, 8])
    brt_b = din("brt_b", [128, 36])
    wr = din("wr", [128, 16 * 36])
    ident = din("ident", [128, 128])
    ltri = din("ltri", [128, 128])
    utri = din("utri", [128, 128])
    masks = din("masks", [128, 18 * 512])
    sel = din("sel", [128, NCH * 16])
    iota = din("iota", [128, 24])
    jv = din("jv", [128, NBLK * 32])
    out = nc.dram_tensor("out", [NOWN, D], F32, kind="ExternalOutput").ap()
    kT_d = dscr("kT_d", [8 * 128, LP], BF16)
    v_d = dscr("v_d", [8 * 128, NCH * 128], BF16)
    h1_d = dscr("h1_d", [NOWN, D], F32)
    xs_d = dscr("xs_d", [NBLK * 128, D], BF16)
    ys_d = dscr("ys_d", [NBLK * 128, D], F32)
    q_d = dscr("q_d", [8 * 128, NOWN], BF16)
    mix_d = dscr("mix_d", [16 * 128, NOWN], BF16)
    mix_dv = mix_d.rearrange("(c d) t -> d c t", c=16)
    v_dv = v_d.rearrange("(h t) (g d) -> t h g d", h=8, g=NCH)

    with contextlib.ExitStack() as es:
        S = Sched(nc, es)

        def sb(st, n, sh, dt):
            return st.enter_context(nc.sbuf_tensor(n, sh, dt))

        def pt(st, n, sh, dt):
            return st.enter_context(nc.psum_tensor(n, sh, dt))

        V, A, G, PE = nc.vector, nc.scalar, nc.gpsimd, nc.tensor
        R_XS = G.to_reg(NBLK * 128 - 1)
        R_W13 = G.to_reg(32 * 2048 - 1)
        R_W2 = G.to_reg(32 * 1024 - 1)

        ident_f = sb(es, "ident_f", [128, 128], F32)
        ident_b = sb(es, "ident_b", [128, 128], BF16)
        ones_f = sb(es, "ones_f", [128, 128], F32)
        ones_b = sb(es, "ones_b", [128, 128], BF16)
        ltri_b = sb(es, "ltri_b", [128, 128], BF16)
        ltri_f = sb(es, "ltri_f", [128, 128], F32)
        utri_f = sb(es, "utri_f", [128, 128], F32)
        gT_s = sb(es, "gT_s", [128, 16], F32)
        bT_s = sb(es, "bT_s", [128, 16], F32)
        iota_s = sb(es, "iota_s", [128, 24], F32)
        stt = sb(es, "stt", [128, 2 * 24], F32)
        mvt = sb(es, "mvt", [128, 2 * 4], F32)
        LG = sb(es, "LG", [128, 16 * 36], F32)
        GATE = sb(es, "GATE", [128, 16 * 2], F32)
        DEST = sb(es, "DEST", [128, 16 * 2], I32)
        OHB = sb(es, "OHB", [128, 16 * 32], BF16)
        OH12 = sb(es, "OH12", [128, 16 * 64], F32)
        IDX13 = sb(es, "IDX13", [128, NBLK * 16], I32)
        IDX2 = sb(es, "IDX2", [128, NBLK * 8], I32)

        def cload(dst, src, key):
            S.dma('sp', lambda: nc.sync.dma_start(out=dst, in_=src), 'cst', writes=[key])
        cload(ident_f[:], ident[:, :], 'ident_f')
        cload(ltri_f[:], ltri[:, :], 'ltri_f')
        cload(utri_f[:], utri[:, :], 'utri_f')
        cload(gT_s[:], gT[:, :], 'gT')
        cload(bT_s[:], bT[:, :], 'bT')
        tk = None
        S.dma('sp', lambda: nc.sync.dma_start(out=iota_s[:], in_=iota[:, :]), 'cst', writes=['iota'])
        S.regroup(S.lastw.get('iota'), ['ident_f', 'ltri_f', 'utri_f', 'gT', 'bT', 'iota'])
        S.op('dve', lambda: V.tensor_copy(out=ident_b[:], in_=ident_f[:]), reads=['ident_f'], writes=['ident_b'])
        S.op('dve', lambda: V.tensor_copy(out=ltri_b[:], in_=ltri_f[:]), reads=['ltri_f'], writes=['ltri_b'])
        S.op('dve', lambda: V.memset(ones_f[:], 1.0), writes=['ones_f'])
        S.op('dve', lambda: V.memset(ones_b[:], 1.0), writes=['ones_b'])
        if stop == 0.1:
            S.muted = True

        def ln_stats(src, kin, i):
            b = i % 2
            st = stt[:, b * 24:(b + 1) * 24]
            for q in range(4):
                S.op('dve', lambda q=q: V.bn_stats(out=st[:, q * 6:(q + 1) * 6], in_=src[:, q * 512:(q + 1) * 512]),
                     reads=[kin], writes=[('st', b, q)])
            S.op('dve', lambda: V.bn_aggr(out=mvt[:, b * 4:b * 4 + 2], in_=st),
                 reads=[('st', b, q) for q in range(4)], writes=[('mv', b)])
            S.op('act', lambda: A.activation(out=mvt[:, b * 4 + 2:b * 4 + 3], in_=mvt[:, b * 4 + 1:b * 4 + 2],
                                             func=AF.Sqrt, bias=EPS), reads=[('mv', b)], writes=[('sd', b)])
            S.op('dve', lambda: V.reciprocal(out=mvt[:, b * 4 + 3:b * 4 + 4], in_=mvt[:, b * 4 + 2:b * 4 + 3]),
                 reads=[('sd', b)], writes=[('rs', b)])
            return mvt[:, b * 4:b * 4 + 1], mvt[:, b * 4 + 3:b * 4 + 4], [('mv', b), ('rs', b)]

        def normalize(dst, src, mean, rstd, reads, writes):
            S.op('dve', lambda: V.tensor_scalar(out=dst, in0=src, scalar1=mean, scalar2=rstd,
                                                op0=ALU.subtract, op1=ALU.mult), reads=reads, writes=writes)

        with contextlib.ExitStack() as esA:
            c_all = sb(esA, "c_all", [128, NCH * 8], F32)
            CJ = sb(esA, "CJ", [128, 128], F32)

            with contextlib.ExitStack() as e1:
                Wb = sb(e1, "Wb", [128, 16 * 2056], BF16)
                xin = [sb(e1, "xin%d" % i, [128, D], F32) for i in range(2)]
                hT = [sb(e1, "hT%d" % i, [128, 16 * 640], BF16) for i in range(2)]
                psc = sb(e1, "psc", [128, 8], F32)
                e1a = contextlib.ExitStack()
                kst = [sb(e1a, "kst%d" % i, [128, 512], BF16) for i in range(2)]
                vst = [sb(e1a, "vst%d" % i, [128, 1024], BF16) for i in range(2)]
                lf_all = sb(e1a, "lf_all", [128, NCH * 8], F32)
                carry = sb(e1a, "carry", [128, NCH * 8], F32)
                tot_s = sb(e1a, "tot_s", [128, NCH * 8], F32)
                zt = sb(e1a, "zt", [128, 16], F32)
                bf_s = sb(e1a, "bf_s", [128, 8], F32)
                sel_s = sb(e1a, "sel_s", [128, NCH * 16], F32)
                Rt = sb(e1a, "Rt", [128, 2 * 128], F32)
                pst = pt(e1, "pst", [128, 1024], F32)
                pa = [pt(e1, "pa%d" % i, [128, 512], F32) for i in range(6)]

                cload(bf_s[:], bf_b[:, :], 'bf_s')
                cload(sel_s[:], sel[:, :], 'sel_s')
                cload(psc[:], pscale[:, :], 'psc')
                S.regroup(S.lastw.get('psc'), ['bf_s', 'sel_s', 'psc'])

                def load_w(cols):
                    keys = []
                    t = None
                    for dc in range(16):
                        for (c0, n, d0) in cols:
                            t = S.dma('pool', lambda dc=dc, c0=c0, n=n, d0=d0: G.dma_start(
                                out=Wb[:, dc * 2056 + d0: dc * 2056 + d0 + n],
                                in_=w_in[dc * 128:(dc + 1) * 128, c0:c0 + n]), 'wld', writes=['Wb'])
                    S.regroup(t, ['Wb'])

                def front(src_ap, i, hbuf, hstride, hcol):
                    b = i % 2
                    S.dma('sp', lambda: nc.sync.dma_start(out=xin[b][:], in_=src_ap), 'xin%d' % b, writes=[('xin', b)])
                    mean, rstd, rk = ln_stats(xin[b], ('xin', b), i)
                    normalize(xin[b][:], xin[b][:], mean, rstd, [('xin', b)] + rk, [('xin', b)])
                    for r4 in range(4):
                        bk = r4 % 2
                        for k4 in range(4):
                            dc = r4 * 4 + k4
                            S.op('pe', lambda dc=dc, bk=bk, k4=k4: PE.transpose(pst[:, bk * 512 + k4 * 128: bk * 512 + (k4 + 1) * 128],
                                                                                xin[b][:, dc * 128:(dc + 1) * 128], ident_f[:]),
                                 reads=[('xin', b), 'ident_f'], writes=[('pst', bk)])
                        for k4 in range(4):
                            dc = r4 * 4 + k4
                            S.op('act', lambda dc=dc, bk=bk, k4=k4: A.activation(out=hbuf[:, dc * hstride + hcol: dc * hstride + hcol + 128],
                                                                                 in_=pst[:, bk * 512 + k4 * 128: bk * 512 + (k4 + 1) * 128], func=AF.Identity,
                                                                                 scale=gT_s[:, dc:dc + 1], bias=bT_s[:, dc:dc + 1]),
                                 reads=[('pst', bk), 'gT', 'bT'], writes=[('hT', id(hbuf), hcol, dc)])
                    if stop == 0.45:
                        S.muted = True

                scope = es.enter_context(contextlib.ExitStack())
                def SC(name):
                    scope.close()
                    scope.enter_context(nc.named_scope(name))
                SC("s1a")
                load_w([(1024, 2056, 0)])
                if stop == 0.2:
                    S.muted = True
                tiles = [(i * 512, 512) for i in range(8)] + [(4096, 128)]
                gi = 0
                for ti, (t0, TW) in enumerate(tiles):
                    hb = hT[ti % 2]
                    for cc in range(TW // 128):
                        g = t0 // 128 + cc
                        front(xfull[g * 128:(g + 1) * 128, :], gi, hb, 640, cc * 128)
                        gi += 1
                        if stop == 0.45:
                            S.muted = True
                        if stop == 0.5:
                            S.dma('sp', lambda: nc.sync.dma_start(out=dbgb[:, :], in_=hb[:]), 'dbg', reads=hkd)
                            S.muted = True
                        hk = ('hT', id(hb), cc * 128)
                        hkd = [('hT', id(hb), cc * 128, dc_) for dc_ in range(16)]
                        for dc in range(16):
                            lhs = hb[:, dc * 640 + cc * 128: dc * 640 + cc * 128 + 128]
                            S.op('pe', lambda dc=dc, lhs=lhs: PE.matmul(pa[0][:], lhsT=lhs, rhs=Wb[:, dc * 2056 + 1024: dc * 2056 + 1536],
                                                                        start=(dc == 0), stop=(dc == 15)), reads=[hkd[dc], 'Wb'], writes=['pv0'])
                            S.op('pe', lambda dc=dc, lhs=lhs: PE.matmul(pa[1][:], lhsT=lhs, rhs=Wb[:, dc * 2056 + 1536: dc * 2056 + 2048],
                                                                        start=(dc == 0), stop=(dc == 15)), reads=[hkd[dc], 'Wb'], writes=['pv1'])
                            S.op('pe', lambda dc=dc, lhs=lhs: PE.matmul(pa[2][:, 0:8], lhsT=lhs, rhs=Wb[:, dc * 2056 + 2048: dc * 2056 + 2056],
                                                                        start=(dc == 0), stop=(dc == 15)), reads=[hkd[dc], 'Wb'], writes=['pf'])
                        vb = g % 2
                        S.op('act', lambda vb=vb: A.copy(out=vst[vb][:, 0:512], in_=pa[0][:]), reads=['pv0'], writes=[('vst', vb, 0)])
                        S.op('dve', lambda vb=vb: V.tensor_copy(out=vst[vb][:, 512:1024], in_=pa[1][:]), reads=['pv1'], writes=[('vst', vb, 1)])
                        S.dma('sp', lambda vb=vb, g=g: nc.sync.dma_start(out=v_dv[:, :, g, :], in_=vst[vb][:].rearrange("t (h d) -> t h d", h=8)),
                              'vst%d' % vb, reads=[('vst', vb, 0), ('vst', vb, 1)], writes=[('v_d', g)])
                        S.op('dve', lambda: V.tensor_tensor(out=zt[:, 0:8], in0=pa[2][:, 0:8], in1=bf_s[:], op=ALU.add),
                             reads=['pf', 'bf_s'], writes=['zt0'])
                        S.op('act', lambda: A.activation(out=zt[:, 8:16], in_=zt[:, 0:8], func=AF.Exp, scale=-1.0), reads=['zt0'], writes=['zt1'])
                        S.op('act', lambda: A.activation(out=zt[:, 0:8], in_=zt[:, 8:16], func=AF.Ln, bias=1.0), reads=['zt1'], writes=['zt0'])
                        S.op('dve', lambda g=g: V.tensor_scalar(out=lf_all[:, g * 8:(g + 1) * 8], in0=zt[:, 0:8], scalar1=-1.0, scalar2=None,
                                                                op0=ALU.mult), reads=['zt0'], writes=['lf'])
                    hks = [('hT', id(hb), cc * 128, dc_) for cc in range(TW // 128) for dc_ in range(16)]
                    for h in range(8):
                        pk = pa[3 + h % 2]
                        for dc in range(16):
                            S.op('pe', lambda dc=dc, h=h, pk=pk: PE.matmul(pk[:, 0:TW], lhsT=Wb[:, dc * 2056 + h * 128: dc * 2056 + h * 128 + 128],
                                                                           rhs=hb[:, dc * 640: dc * 640 + TW], start=(dc == 0), stop=(dc == 15)),
                                 reads=hks + ['Wb'], writes=[('pk', h % 2)])
                        kb = h % 2
                        if h % 2 == 0:
                            S.op('act', lambda pk=pk, kb=kb: A.copy(out=kst[kb][:, 0:TW], in_=pk[:, 0:TW]), reads=[('pk', kb)], writes=[('kst', kb)])
                        else:
                            S.op('dve', lambda pk=pk, kb=kb: V.tensor_copy(out=kst[kb][:, 0:TW], in_=pk[:, 0:TW]), reads=[('pk', kb)], writes=[('kst', kb)])
                        S.dma('sp', lambda h=h, kb=kb: nc.sync.dma_start(out=kT_d[h * 128:(h + 1) * 128, t0:t0 + TW], in_=kst[kb][:, 0:TW]),
                              'kst%d' % kb, reads=[('kst', kb)], writes=[('kT_d', h)])
                    if stop == 0.6:
                        S.muted = True

                if stop == 0.8:
                    S.dma('sp', lambda: nc.sync.dma_start(out=dbg[:, 0:264], in_=lf_all[:]), 'dbg')
                    S.muted = True
                SC("s1c")
                S.op('pe', lambda: PE.matmul(pa[0][:, 0:264], lhsT=ones_f[:], rhs=lf_all[:], start=True, stop=True), reads=['lf', 'ones_f'], writes=['pv0'])
                S.op('pe', lambda: PE.matmul(pa[1][:, 0:264], lhsT=utri_f[:], rhs=lf_all[:], start=True, stop=True), reads=['lf', 'utri_f'], writes=['pv1'])
                S.op('dve', lambda: V.tensor_copy(out=tot_s[:], in_=pa[0][:, 0:264]), reads=['pv0'], writes=['tot'])
                S.op('dve', lambda: V.memset(carry[:, 0:8], 0.0), writes=['carry'])
                for g in range(1, NCH):
                    S.op('dve', lambda g=g: V.tensor_tensor(out=carry[:, g * 8:(g + 1) * 8], in0=carry[:, (g - 1) * 8:g * 8],
                                                            in1=tot_s[:, (g - 1) * 8:g * 8], op=ALU.add), reads=['carry', 'tot'], writes=['carry'])
                S.op('dve', lambda: V.tensor_tensor(out=c_all[:], in0=pa[1][:, 0:264], in1=carry[:], op=ALU.add), reads=['pv1', 'carry'], writes=['c_all'])
                for g in range(NCH):
                    rb = g % 2
                    S.op('dve', lambda g=g, rb=rb: V.tensor_tensor(
                        out=Rt[:, rb * 128:(rb + 1) * 128].rearrange("p (h j) -> p h j", h=8),
                        in0=c_all[:, g * 8:(g + 1) * 8].unsqueeze(2).to_broadcast([128, 8, 16]),
                        in1=sel_s[:, g * 16:(g + 1) * 16].unsqueeze(1).to_broadcast([128, 8, 16]), op=ALU.mult),
                        reads=['c_all', 'sel_s'], writes=[('Rt', rb)])
                    S.op('pe', lambda g=g, rb=rb: PE.matmul(pa[2][:, 0:128], lhsT=ones_f[:], rhs=Rt[:, rb * 128:(rb + 1) * 128],
                                                            start=(g == 0), stop=(g == NCH - 1)), reads=[('Rt', rb), 'ones_f'], writes=['pf'])
                S.op('dve', lambda: V.tensor_copy(out=CJ[:], in_=pa[2][:, 0:128]), reads=['pf'], writes=['CJ'])

                S.barrier()
                if stop == 0.9:
                    S.dma('sp', lambda: nc.sync.dma_start(out=dbg[:, 0:264], in_=c_all[:]), 'dbg')
                    S.dma('sp', lambda: nc.sync.dma_start(out=dbg[:, 512:640], in_=CJ[:]), 'dbg')
                    S.muted = True
                e1a.close()
                SC("s1b")
                U = sb(e1, "U", [128, 8 * 640], F32)
                UA = sb(e1, "UA", [128, 2 * 640], F32)
                UB = sb(e1, "UB", [128, 2 * 640], F32)
                PL = sb(e1, "PL", [128, 8 * 512], BF16)
                PW = sb(e1, "PW", [128, 8 * 256], BF16)
                qst = [sb(e1, "qst%d" % i, [128, 512], BF16) for i in range(2)]
                load_w([(0, 1024, 0), (3080, 1024, 1024)])
                keys = []
                for gq in range(8):
                    S.dma('pool', lambda gq=gq: G.dma_start(out=PW[:, gq * 256:(gq + 1) * 256], in_=pool_w[gq * 128:(gq + 1) * 128, :]), 'pwl', writes=['PW'])
                S.regroup(S.lastw.get('PW'), ['PW'])
                for p in range(4):
                    hb = hT[p % 2]
                    for cc in range(5):
                        front(xown[p * 640 + cc * 128: p * 640 + (cc + 1) * 128, :], gi, hb, 640, cc * 128)
                        gi += 1
                    hks = [('hT', id(hb), cc * 128, dc_) for cc in range(5) for dc_ in range(16)]
                    for h in range(8):
                        pk = pa[h % 2]
                        for dc in range(16):
                            S.op('pe', lambda dc=dc, h=h, pk=pk: PE.matmul(pk[:], lhsT=Wb[:, dc * 2056 + h * 128: dc * 2056 + h * 128 + 128],
                                                                           rhs=hb[:, dc * 640 + 128: dc * 640 + 640], start=(dc == 0), stop=(dc == 15)),
                                 reads=hks + ['Wb'], writes=[('pq', h % 2)])
                        dst = qst[h % 2][:]
                        if h % 2 == 0:
                            S.op('act', lambda pk=pk, dst=dst: A.copy(out=dst, in_=pk[:]), reads=[('pq', 0)], writes=[('qst', 0)])
                        else:
                            S.op('dve', lambda pk=pk, dst=dst: V.tensor_copy(out=dst, in_=pk[:]), reads=[('pq', 1)], writes=[('qst', 1)])
                        S.dma('sp', lambda h=h, dst=dst: nc.sync.dma_start(out=q_d[h * 128:(h + 1) * 128, p * 512:(p + 1) * 512], in_=dst), 'kst%d' % (h % 2),
                              reads=[('qst', h % 2)], writes=[('q_d', h, p)])
                    for ch in range(8):
                        pA, pB = pa[2 + 2 * (ch % 2)], pa[3 + 2 * (ch % 2)]
                        for dc in range(16):
                            lhs = Wb[:, dc * 2056 + 1024 + ch * 128: dc * 2056 + 1024 + ch * 128 + 128]
                            S.op('pe', lambda dc=dc, lhs=lhs, pA=pA: PE.matmul(pA[:], lhsT=lhs, rhs=hb[:, dc * 640 + 128: dc * 640 + 640],
                                                                               start=(dc == 0), stop=(dc == 15)), reads=hks + ['Wb'], writes=[('pu', ch % 2, 0)])
                            S.op('pe', lambda dc=dc, lhs=lhs, pB=pB: PE.matmul(pB[:, 0:128], lhsT=lhs, rhs=hb[:, dc * 640: dc * 640 + 128],
                                                                               start=(dc == 0), stop=(dc == 15)), reads=hks + ['Wb'], writes=[('pu', ch % 2, 1)])
                        S.op('act', lambda ch=ch, pA=pA: A.copy(out=U[:, ch * 640 + 128:(ch + 1) * 640], in_=pA[:]), reads=[('pu', ch % 2, 0)], writes=[('U', ch)])
                        S.op('act', lambda ch=ch, pB=pB: A.copy(out=U[:, ch * 640:ch * 640 + 128], in_=pB[:, 0:128]), reads=[('pu', ch % 2, 1)], writes=[('U', ch)])
                    for ch in range(8):
                        gg = ch // 2
                        eng, E = ('dve', V) if ch % 2 == 0 else ('pool', G)
                        sl = ch % 2
                        u = U[:, ch * 640:(ch + 1) * 640]
                        a = UA[:, sl * 640:(sl + 1) * 640]
                        bq = UB[:, sl * 640:(sl + 1) * 640]
                        cur, curk = u, ('U', ch)
                        lo, st = 0, 1
                        bufs = [(a, ('UA', sl)), (bq, ('UB', sl))]
                        for step in range(gg + 1):
                            dstb, dk = bufs[step % 2]
                            lo2 = lo + st
                            S.op(eng, lambda E=E, dstb=dstb, cur=cur, lo2=lo2, st=st: E.tensor_tensor(
                                out=dstb[:, lo2:640], in0=cur[:, lo2:640], in1=cur[:, lo2 - st:640 - st], op=ALU.add),
                                reads=[curk], writes=[dk])
                            cur, curk, lo, st = dstb, dk, lo2, st * 2
                        w = float(2 ** (gg + 1))
                        S.op('dve', lambda cur=cur, u=u, ch=ch, w=w: V.scalar_tensor_tensor(
                            out=PL[:, ch * 512:(ch + 1) * 512], in0=cur[:, 128:640], scalar=1.0 / w, in1=u[:, 128:640],
                            op0=ALU.mult, op1=ALU.subtract), reads=[curk, ('U', ch)], writes=[('PL', ch)])
                    for gg in range(4):
                        for dco in range(2):
                            pk = pa[(gg * 2 + dco) % 2]
                            for ci in range(2):
                                S.op('pe', lambda gg=gg, dco=dco, ci=ci, pk=pk: PE.matmul(
                                    pk[:], lhsT=PW[:, (gg * 2 + ci) * 256 + dco * 128:(gg * 2 + ci) * 256 + dco * 128 + 128],
                                    rhs=PL[:, (gg * 2 + ci) * 512:(gg * 2 + ci + 1) * 512], start=(ci == 0), stop=(ci == 1)),
                                    reads=[('PL', gg * 2), ('PL', gg * 2 + 1), 'PW'], writes=[('pq', (gg * 2 + dco) % 2)])
                            oc = 8 + gg * 2 + dco
                            qb_ = (gg * 2 + dco) % 2
                            S.op('act', lambda pk=pk, gg=gg, dco=dco, qb_=qb_: A.activation(
                                out=qst[qb_][:], in_=pk[:], func=AF.Identity,
                                scale=psc[:, gg * 2 + dco: gg * 2 + dco + 1]), reads=[('pq', qb_), 'psc'], writes=[('qst', qb_)])
                            S.dma('sp', lambda oc=oc, qb_=qb_: nc.sync.dma_start(out=mix_d[oc * 128:(oc + 1) * 128, p * 512:(p + 1) * 512], in_=qst[qb_][:]),
                                  'kst%d' % qb_, reads=[('qst', qb_)], writes=[('mix_d', oc, p)])
                S.barrier()
                if stop == 1:
                    S.dma('sp', lambda: nc.sync.dma_start(out=dbg[:, 0:264], in_=c_all[:]), 'dbg')
                    S.dma('sp', lambda: nc.sync.dma_start(out=dbg[:, 512:640], in_=CJ[:]), 'dbg')
                    S.muted = True

            with contextlib.ExitStack() as e2:
                SC("s2")
                kTb = [sb(e2, "kTb%d" % i, [128, LP], BF16) for i in range(2)]
                Vb = [sb(e2, "Vb%d" % i, [128, NCH * 128], BF16) for i in range(2)]
                PT = [sb(e2, "PT%d" % i, [128, 512], BF16) for i in range(6)]
                MKf = sb(e2, "MKf", [128, 512], F32)
                MK = sb(e2, "MK", [128, 18 * 512], BF16)
                BT = sb(e2, "BT", [128, 8 * NCH * 16], F32)
                Qb = [sb(e2, "Qb%d" % i, [128, NOWN], BF16) for i in range(2)]
                ost = [sb(e2, "ost%d" % i, [128, 512], BF16) for i in range(2)]
                rden = sb(e2, "rden", [128, 512], F32)
                SL = [sb(e2, "SL%d" % i, [128, 512], F32) for i in range(4)]
                ps_s = [pt(e2, "ps_s%d" % i, [128, 512], F32) for i in range(4)]
                ps_o = [pt(e2, "ps_o%d" % i, [128, 512], F32) for i in range(2)]
                ps_d = [pt(e2, "ps_d%d" % i, [128, 512], F32) for i in range(2)]
                for h in range(8):
                    S.op('dve', lambda h=h: V.tensor_tensor(
                        out=BT[:, h * NCH * 16:(h + 1) * NCH * 16].rearrange("p (g j) -> p g j", g=NCH),
                        in0=CJ[:, h * 16:(h + 1) * 16].unsqueeze(1).to_broadcast([128, NCH, 16]),
                        in1=c_all[:].rearrange("p (g h) -> p g h", h=8)[:, :, h:h + 1].to_broadcast([128, NCH, 16]),
                        op=ALU.subtract), reads=['CJ', 'c_all'], writes=['BT'])
                S.op('dve', lambda: V.tensor_scalar(out=BT[:], in0=BT[:], scalar1=0.0, scalar2=None, op0=ALU.min), reads=['BT'], writes=['BT'])
                for i in range(18):
                    S.dma('sp', lambda i=i: nc.sync.dma_start(out=MKf[:], in_=masks[:, i * 512:(i + 1) * 512]), 'mk', writes=['MKf'])
                    S.op('dve', lambda i=i: V.tensor_copy(out=MK[:, i * 512:(i + 1) * 512], in_=MKf[:]), reads=['MKf'], writes=['MK'])
                def load_head(h):
                    hb_ = h % 2
                    S.dma('sp', lambda: nc.sync.dma_start(out=kTb[hb_][:], in_=kT_d[h * 128:(h + 1) * 128, :]), 'kl%d' % hb_,
                          reads=[('kT_d', h)], writes=[('kTb', hb_)])
                    S.dma('sp', lambda: nc.sync.dma_start(out=Vb[hb_][:], in_=v_d[h * 128:(h + 1) * 128, :]), 'vl%d' % hb_,
                          reads=[('v_d', g) for g in range(NCH)], writes=[('Vb', hb_)])
                    S.dma('sp', lambda: nc.sync.dma_start(out=Qb[hb_][:], in_=q_d[h * 128:(h + 1) * 128, :]), 'ql%d' % hb_,
                          reads=[('q_d', h, p) for p in range(4)], writes=[('Qb', hb_)])

                its = [(h, p, kc) for h in range(8) for p in range(4) for kc in range(8 * p + 9)]

                def s_step(i):
                    h, p, kc = its[i]
                    hb_, sbk = h % 2, i % 4
                    S.op('pe', lambda: PE.matmul(ps_s[sbk][:], lhsT=kTb[hb_][:, kc * 128:(kc + 1) * 128], rhs=Qb[hb_][:, p * 512:(p + 1) * 512],
                                                 start=True, stop=True), reads=[('kTb', hb_), ('Qb', hb_)], writes=[('ps_s', sbk)])

                def e_step(i):
                    h, p, kc = its[i]
                    sbk, pb = i % 4, i % 6
                    bcol = (h * NCH + kc) * 16 + 4 * p
                    S.op('dve', lambda: V.scalar_tensor_tensor(
                        out=SL[sbk][:].rearrange("k (j q) -> k j q", j=4), in0=ps_s[sbk][:].rearrange("k (j q) -> k j q", j=4), scalar=SCALE,
                        in1=BT[:, bcol:bcol + 4].unsqueeze(2).to_broadcast([128, 4, 128]), op0=ALU.mult, op1=ALU.add),
                        reads=[('ps_s', sbk), 'BT'], writes=[('SL', sbk)])
                    S.op('act', lambda: A.activation(out=PT[pb][:], in_=SL[sbk][:], func=AF.Exp), reads=[('SL', sbk)], writes=[('PT', pb)])
                    if kc >= 8 * p:
                        mi = (p % 2) * 9 + kc - 8 * p
                        if kc % 2 == 0:
                            S.op('pool', lambda: G.tensor_tensor(out=PT[pb][:], in0=PT[pb][:], in1=MK[:, mi * 512:(mi + 1) * 512], op=ALU.mult),
                                 reads=[('PT', pb), 'MK'], writes=[('PT', pb)])
                        else:
                            S.op('dve', lambda: V.tensor_tensor(out=PT[pb][:], in0=PT[pb][:], in1=MK[:, mi * 512:(mi + 1) * 512], op=ALU.mult),
                                 reads=[('PT', pb), 'MK'], writes=[('PT', pb)])

                def pv_step(i):
                    h, p, kc = its[i]
                    hb_, pb = h % 2, i % 6
                    E_ = 8 * p + 9
                    ob = (h * 4 + p) % 2
                    S.op('pe', lambda: PE.matmul(ps_o[ob][:], lhsT=Vb[hb_][:, kc * 128:(kc + 1) * 128], rhs=PT[pb][:],
                                                 start=(kc == 0), stop=(kc == E_ - 1)), reads=[('Vb', hb_), ('PT', pb)], writes=[('ps_o', ob)])
                    S.op('pe', lambda: PE.matmul(ps_d[ob][:], lhsT=ones_b[:], rhs=PT[pb][:],
                                                 start=(kc == 0), stop=(kc == E_ - 1)), reads=['ones_b', ('PT', pb)], writes=[('ps_d', ob)])
                    if kc == E_ - 1:
                        S.op('dve', lambda: V.reciprocal(out=rden[:], in_=ps_d[ob][:]), reads=[('ps_d', ob)], writes=['rden'])
                        S.op('dve', lambda: V.tensor_tensor(out=ost[ob][:], in0=ps_o[ob][:],
                                                            in1=rden[:], op=ALU.mult), reads=[('ps_o', ob), 'rden'], writes=[('ost', ob)])
                        S.dma('sp', lambda: nc.sync.dma_start(out=mix_d[h * 128:(h + 1) * 128, p * 512:(p + 1) * 512], in_=ost[ob][:]),
                              'ost%d' % ob, reads=[('ost', ob)], writes=[('mix_d', h, p)])

                load_head(0)
                load_head(1)
                LA = 3
                for i in range(LA):
                    s_step(i)
                for i in range(len(its)):
                    h, p, kc = its[i]
                    if i + LA < len(its):
                        s_step(i + LA)
                    e_step(i)
                    pv_step(i)
                    if p == 3 and kc == 8 * p + 8 and h + 2 < 8:
                        load_head(h + 2)
                S.barrier()
                if stop == 2:
                    S.muted = True

            with contextlib.ExitStack() as e3:
                SC("s3")
                WO = sb(e3, "WO", [128, 16 * D], BF16)
                gb = sb(e3, "gb_s", [128, 4 * D], F32)
                xb = [sb(e3, "xb%d" % i, [128, D], F32) for i in range(2)]
                mxt = [sb(e3, "mxt%d" % i, [128, 16 * 512], BF16) for i in range(2)]
                h1T = sb(e3, "h1T", [128, 16 * 128], F32)
                WR = sb(e3, "WR", [128, 16 * 36], F32)
                brt = sb(e3, "brt", [128, 36], F32)
                pm = [pt(e3, "pm%d" % i, [128, 512], F32) for i in range(4)]
                ptr = [pt(e3, "ptr%d" % i, [128, 512], F32) for i in range(2)]
                plg = pt(e3, "plg", [128, 512], F32)
                for dc in range(16):
                    S.dma('pool', lambda dc=dc: G.dma_start(out=WO[:, dc * D:(dc + 1) * D], in_=w_out[dc * 128:(dc + 1) * 128, :]), 'wld', writes=['WO'])
                S.regroup(S.lastw.get('WO'), ['WO'])
                S.dma('sp', lambda: nc.sync.dma_start(out=gb[:], in_=gb_d[:, 0:4 * D]), 'cst', writes=['gb'])
                S.dma('sp', lambda: nc.sync.dma_start(out=WR[:], in_=wr[:, :]), 'cst', writes=['WR'])
                S.dma('sp', lambda: nc.sync.dma_start(out=brt[:], in_=brt_b[:, :]), 'cst', writes=['brt'])
                S.regroup(S.lastw.get('brt'), ['gb', 'WR', 'brt'])
                def s3_load(c):
                    b, p, cc = c % 2, c // 4, c % 4
                    if cc == 0:
                        S.dma('sp', lambda: nc.sync.dma_start(out=mxt[p % 2][:].rearrange("d (c t) -> d c t", c=16), in_=mix_dv[:, :, p * 512:(p + 1) * 512]), 'mxl%d' % (p % 2),
                              reads=[('mix_d', oc_, p) for oc_ in range(16)], writes=[('mxt', p % 2)])
                    S.dma('sp', lambda: nc.sync.dma_start(out=xb[b][:], in_=xown[p * 640 + 128 + cc * 128: p * 640 + 256 + cc * 128, :]), 'xin%d' % b,
                          writes=[('xb', b)])

                def s3_mm(c):
                    p, cc = c // 4, c % 4
                    mx = mxt[p % 2]
                    for fb in range(4):
                        for dc in range(16):
                            S.op('pe', lambda fb=fb, dc=dc: PE.matmul(pm[fb][:], lhsT=mx[:, dc * 512 + cc * 128: dc * 512 + (cc + 1) * 128],
                                                                      rhs=WO[:, dc * D + fb * 512: dc * D + (fb + 1) * 512], start=(dc == 0), stop=(dc == 15)),
                                 reads=[('mxt', p % 2), 'WO'], writes=[('pm', fb)])

                def s3_resid(c, gi):
                    b = c % 2
                    x_ = xb[b]
                    mean, rstd, rk = ln_stats(x_, ('xb', b), gi)
                    normalize(x_[:], x_[:], mean, rstd, [('xb', b)] + rk, [('xb', b)])
                    S.op('pool', lambda: G.tensor_tensor(out=x_[:], in0=x_[:], in1=gb[:, 0:D], op=ALU.mult), reads=[('xb', b), 'gb'], writes=[('xb', b)])
                    S.op('pool', lambda: G.tensor_tensor(out=x_[:], in0=x_[:], in1=gb[:, D:2 * D], op=ALU.add), reads=[('xb', b), 'gb'], writes=[('xb', b)])
                    for fb in range(4):
                        S.op('dve', lambda fb=fb: V.scalar_tensor_tensor(out=x_[:, fb * 512:(fb + 1) * 512], in0=x_[:, fb * 512:(fb + 1) * 512], scalar=ALPHA,
                                                                         in1=pm[fb][:], op0=ALU.mult, op1=ALU.add), reads=[('xb', b), ('pm', fb)], writes=[('xb', b)])

                def s3_post(c, gi):
                    b = c % 2
                    x_ = xb[b]
                    mean, rstd, rk = ln_stats(x_, ('xb', b), gi)
                    normalize(x_[:], x_[:], mean, rstd, [('xb', b)] + rk, [('xb', b)])
                    S.op('pool', lambda: G.tensor_tensor(out=x_[:], in0=x_[:], in1=gb[:, 2 * D:3 * D], op=ALU.mult), reads=[('xb', b), 'gb'], writes=[('xb', b)])
                    S.op('dve', lambda: V.tensor_tensor(out=x_[:], in0=x_[:], in1=gb[:, 3 * D:4 * D], op=ALU.add), reads=[('xb', b), 'gb'], writes=[('xb', b)])
                    S.dma('sp', lambda: nc.sync.dma_start(out=h1_d[c * 128:(c + 1) * 128, :], in_=x_[:]), 'h1s%d' % b, reads=[('xb', b)], writes=[('h1_d', c)])
                    for q4 in range(4):
                        for k4 in range(4):
                            dc = q4 * 4 + k4
                            S.op('pe', lambda dc=dc, q4=q4, k4=k4: PE.transpose(ptr[q4 % 2][:, k4 * 128:(k4 + 1) * 128], x_[:, dc * 128:(dc + 1) * 128], ident_f[:]),
                                 reads=[('xb', b), 'ident_f'], writes=[('ptr', q4 % 2)])
                        if q4 % 2 == 0:
                            S.op('act', lambda q4=q4: A.copy(out=h1T[:, q4 * 512:(q4 + 1) * 512], in_=ptr[q4 % 2][:]), reads=[('ptr', q4 % 2)], writes=['h1T'])
                        else:
                            S.op('dve', lambda q4=q4: V.tensor_copy(out=h1T[:, q4 * 512:(q4 + 1) * 512], in_=ptr[q4 % 2][:]), reads=[('ptr', q4 % 2)], writes=['h1T'])
                    for dc in range(16):
                        S.op('pe', lambda dc=dc: PE.matmul(plg[:, 0:36], lhsT=h1T[:, dc * 128:(dc + 1) * 128], rhs=WR[:, dc * 36:(dc + 1) * 36],
                                                           start=(dc == 0), stop=(dc == 15)), reads=['h1T', 'WR'], writes=['plg'])
                    S.op('dve', lambda: V.tensor_tensor(out=LG[:, c * 36:(c + 1) * 36], in0=plg[:, 0:36], in1=brt[:], op=ALU.add), reads=['plg', 'brt'], writes=[('LG', c)])

                s3_load(0)
                s3_mm(0)
                for c in range(16):
                    if c + 1 < 16:
                        s3_load(c + 1)
                    s3_resid(c, gi)
                    gi += 1
                    if c + 1 < 16:
                        s3_mm(c + 1)
                    s3_post(c, gi)
                    gi += 1
                S.barrier()
        S.barrier()
        if stop == 3:
            S.dma('sp', lambda: nc.sync.dma_start(out=dbg[:, 0:16 * 36], in_=LG[:]), 'dbg')
            S.muted = True

        with contextlib.ExitStack() as e4:
            SC("s4")
            tmp = sb(e4, "tmp", [128, 128], F32)
            ohg = sb(e4, "ohg", [128, 4], F32)
            les = sb(e4, "les", [128, 16], F32)
            oh1 = sb(e4, "oh1", [128, 16], F32)
            sc = sb(e4, "sc", [128, 16], F32)
            cnt = sb(e4, "cnt", [128, 32], F32)
            pad_f = sb(e4, "pad_f", [128, 32], F32)
            pad_i = sb(e4, "pad_i", [128, 32], I32)
            pend = sb(e4, "pend", [128, 32], F32)
            pstart = sb(e4, "pstart", [128, 32], F32)
            base = sb(e4, "base", [128, 32], F32)
            jv_s = sb(e4, "jv_sb", [128, NBLK * 32], F32)
            cmp_ = sb(e4, "cmp", [128, NBLK * 32], F32)
            be = sb(e4, "be", [128, NBLK], F32)
            same = sb(e4, "same", [128, NBLK], F32)
            be13 = sb(e4, "be13", [128, NBLK], F32)
            be2 = sb(e4, "be2", [128, NBLK], F32)
            idf = sb(e4, "idf", [128, NBLK * 16], F32)
            dstf = sb(e4, "dstf", [128, 32], F32)
            hbx = [sb(e4, "hbx%d" % i, [128, D], F32) for i in range(2)]
            hbb = [sb(e4, "hbb%d" % i, [128, D], BF16) for i in range(2)]
            ppf = [pt(e4, "ppf%d" % i, [128, 512], F32) for i in range(2)]
            pcn = pt(e4, "pcn", [128, 512], F32)
            S.dma('sp', lambda: nc.sync.dma_start(out=jv_s[:], in_=jv[:, :]), 'cst', writes=['jv_s'])
            for c in range(16):
                lg = LG[:, c * 36: c * 36 + 4]
                le = LG[:, c * 36 + 4: c * 36 + 36]
                S.op('dve', lambda: V.tensor_reduce(out=sc[:, 0:1], in_=lg, axis=AX.X, op=ALU.max), reads=[('LG', c)], writes=['sc0'])
                S.op('dve', lambda: V.tensor_tensor(out=ohg[:], in0=lg, in1=sc[:, 0:1].to_broadcast([128, 4]), op=ALU.is_equal), reads=[('LG', c), 'sc0'], writes=['ohg'])
                S.op('dve', lambda: V.tensor_scalar(out=sc[:, 1:2], in0=sc[:, 0:1], scalar1=-1.0, scalar2=None, op0=ALU.mult), reads=['sc0'], writes=['sc1'])
                S.op('act', lambda: A.activation(out=tmp[:, 0:4], in_=lg, func=AF.Exp, bias=sc[:, 1:2], accum_out=sc[:, 2:3]), reads=[('LG', c), 'sc1'], writes=['sc2', 'tmp'])
                S.op('dve', lambda: V.reciprocal(out=sc[:, 3:4], in_=sc[:, 2:3]), reads=['sc2'], writes=['sc3'])
                S.op('dve', lambda: V.tensor_scalar(out=les[:, 0:8], in0=le[:, 0:8], scalar1=ohg[:, 0:1], scalar2=None, op0=ALU.mult), reads=[('LG', c), 'ohg'], writes=['les'])
                for g in range(1, 4):
                    S.op('dve', lambda g=g: V.scalar_tensor_tensor(out=les[:, 0:8], in0=le[:, g * 8:(g + 1) * 8], scalar=ohg[:, g:g + 1], in1=les[:, 0:8],
                                                                   op0=ALU.mult, op1=ALU.add), reads=[('LG', c), 'ohg', 'les'], writes=['les'])
                S.op('dve', lambda: V.tensor_reduce(out=sc[:, 4:5], in_=les[:, 0:8], axis=AX.X, op=ALU.max), reads=['les'], writes=['sc4'])
                S.op('dve', lambda: V.tensor_tensor(out=oh1[:, 0:8], in0=les[:, 0:8], in1=sc[:, 4:5].to_broadcast([128, 8]), op=ALU.is_equal), reads=['les', 'sc4'], writes=['oh1'])
                S.op('dve', lambda: V.scalar_tensor_tensor(out=les[:, 8:16], in0=oh1[:, 0:8], scalar=-1e30, in1=les[:, 0:8], op0=ALU.mult, op1=ALU.add),
                     reads=['oh1', 'les'], writes=['les2'])
                S.op('dve', lambda: V.tensor_reduce(out=sc[:, 5:6], in_=les[:, 8:16], axis=AX.X, op=ALU.max), reads=['les2'], writes=['sc5'])
                S.op('dve', lambda: V.tensor_tensor(out=oh1[:, 8:16], in0=les[:, 8:16], in1=sc[:, 5:6].to_broadcast([128, 8]), op=ALU.is_equal), reads=['les2', 'sc5'], writes=['oh2'])
                S.op('dve', lambda: V.tensor_tensor(out=sc[:, 6:7], in0=sc[:, 5:6], in1=sc[:, 4:5], op=ALU.subtract), reads=['sc4', 'sc5'], writes=['sc6'])
                S.op('act', lambda: A.activation(out=sc[:, 7:8], in_=sc[:, 6:7], func=AF.Exp), reads=['sc6'], writes=['sc7'])
                S.op('dve', lambda: V.tensor_scalar(out=sc[:, 8:9], in0=sc[:, 7:8], scalar1=1.0, scalar2=None, op0=ALU.add), reads=['sc7'], writes=['sc8'])
                S.op('dve', lambda: V.reciprocal(out=sc[:, 9:10], in_=sc[:, 8:9]), reads=['sc8'], writes=['sc9'])
                S.op('dve', lambda: V.tensor_tensor(out=GATE[:, c * 2:c * 2 + 1], in0=sc[:, 9:10], in1=sc[:, 3:4], op=ALU.mult), reads=['sc9', 'sc3'], writes=[('GATE', c)])
                S.op('dve', lambda: V.tensor_tensor(out=GATE[:, c * 2 + 1:c * 2 + 2], in0=GATE[:, c * 2:c * 2 + 1], in1=sc[:, 7:8], op=ALU.mult), reads=[('GATE', c), 'sc7'], writes=[('GATE', c)])
                for k in range(2):
                    for g in range(4):
                        S.op('dve', lambda k=k, g=g: V.tensor_scalar(out=OH12[:, c * 64 + k * 32 + g * 8: c * 64 + k * 32 + (g + 1) * 8], in0=oh1[:, k * 8:(k + 1) * 8],
                                                                     scalar1=ohg[:, g:g + 1], scalar2=None, op0=ALU.mult), reads=['oh1', 'oh2', 'ohg'], writes=[('OH12', c)])
                S.op('dve', lambda: V.tensor_tensor(out=OHB[:, c * 32:(c + 1) * 32], in0=OH12[:, c * 64: c * 64 + 32], in1=OH12[:, c * 64 + 32: c * 64 + 64], op=ALU.add),
                     reads=[('OH12', c)], writes=[('OHB', c)])
            for c in range(16):
                S.op('pe', lambda c=c: PE.matmul(pcn[:, 0:32], lhsT=ones_b[:], rhs=OHB[:, c * 32:(c + 1) * 32], start=(c == 0), stop=(c == 15)),
                     reads=[('OHB', c), 'ones_b'], writes=['pcn'])
            S.op('dve', lambda: V.tensor_scalar(out=pad_f[:], in0=pcn[:, 0:32], scalar1=127.0, scalar2=None, op0=ALU.add), reads=['pcn'], writes=['pad_f'])
            S.op('dve', lambda: V.tensor_copy(out=pad_i[:], in_=pad_f[:]), reads=['pad_f'], writes=['pad_i'])
            S.op('dve', lambda: V.tensor_scalar(out=pad_i[:], in0=pad_i[:], scalar1=7, scalar2=7, op0=ALU.arith_shift_right, op1=ALU.logical_shift_left),
                 reads=['pad_i'], writes=['pad_i'])
            S.op('dve', lambda: V.tensor_copy(out=pad_f[:], in_=pad_i[:]), reads=['pad_i'], writes=['pad_f'])
            S.op('dve', lambda: V.tensor_copy(out=pend[:, 0:1], in_=pad_f[:, 0:1]), reads=['pad_f'], writes=['pend'])
            for e in range(1, 32):
                S.op('dve', lambda e=e: V.tensor_tensor(out=pend[:, e:e + 1], in0=pend[:, e - 1:e], in1=pad_f[:, e:e + 1], op=ALU.add), reads=['pend', 'pad_f'], writes=['pend'])
            S.op('dve', lambda: V.tensor_tensor(out=pstart[:], in0=pend[:], in1=pad_f[:], op=ALU.subtract), reads=['pend', 'pad_f'], writes=['pstart'])
            S.op('dve', lambda: V.tensor_tensor(out=cmp_[:].rearrange("p (j e) -> p j e", j=NBLK), in0=jv_s[:].rearrange("p (j e) -> p j e", j=NBLK),
                                                in1=pend[:].unsqueeze(1).to_broadcast([128, NBLK, 32]), op=ALU.is_ge), reads=['jv_s', 'pend'], writes=['cmp'])
            S.op('dve', lambda: V.tensor_reduce(out=be[:], in_=cmp_[:].rearrange("p (j e) -> p j e", j=NBLK), axis=AX.X, op=ALU.add), reads=['cmp'], writes=['be'])
            S.op('dve', lambda: V.tensor_scalar(out=be[:], in0=be[:], scalar1=31.0, scalar2=None, op0=ALU.min), reads=['be'], writes=['be'])
            S.op('dve', lambda: V.memset(same[:, 0:1], 0.0), writes=['same'])
            S.op('dve', lambda: V.tensor_tensor(out=same[:, 1:NBLK], in0=be[:, 1:NBLK], in1=be[:, 0:NBLK - 1], op=ALU.is_equal), reads=['be', 'same'], writes=['same'])
            S.op('dve', lambda: V.tensor_scalar(out=same[:], in0=same[:], scalar1=1.0e6, scalar2=None, op0=ALU.mult), reads=['same'], writes=['same'])
            S.op('dve', lambda: V.scalar_tensor_tensor(out=be13[:], in0=be[:], scalar=2048.0, in1=same[:], op0=ALU.mult, op1=ALU.add), reads=['be', 'same'], writes=['be13'])
            S.op('dve', lambda: V.scalar_tensor_tensor(out=be2[:], in0=be[:], scalar=1024.0, in1=same[:], op0=ALU.mult, op1=ALU.add), reads=['be', 'same'], writes=['be2'])
            S.op('dve', lambda: V.tensor_tensor(out=idf[:].rearrange("p (j c) -> p j c", j=NBLK), in0=be13[:].unsqueeze(2).to_broadcast([128, NBLK, 16]),
                                                in1=iota_s[:, 0:16].unsqueeze(1).to_broadcast([128, NBLK, 16]), op=ALU.add),
                 reads=['be13', 'iota'], writes=['idf'])
            S.op('dve', lambda: V.tensor_copy(out=IDX13[:], in_=idf[:]), reads=['idf'], writes=['IDX13'])
            S.op('dve', lambda: V.tensor_tensor(out=idf[:, 0:NBLK * 8].rearrange("p (j c) -> p j c", j=NBLK), in0=be2[:].unsqueeze(2).to_broadcast([128, NBLK, 8]),
                                                in1=iota_s[:, 0:8].unsqueeze(1).to_broadcast([128, NBLK, 8]), op=ALU.add),
                 reads=['be2', 'iota', 'IDX13'], writes=['idf'])
            S.op('dve', lambda: V.tensor_copy(out=IDX2[:], in_=idf[:, 0:NBLK * 8]), reads=['idf'], writes=['IDX2'])
            for c in range(16):
                pp = ppf[c % 2]
                for c2 in range(c):
                    S.op('pe', lambda c2=c2, pp=pp: PE.matmul(pp[:, 0:32], lhsT=ones_b[:], rhs=OHB[:, c2 * 32:(c2 + 1) * 32], start=(c2 == 0), stop=False),
                         reads=[('OHB', c2), 'ones_b'], writes=[('ppf', c % 2)])
                S.op('pe', lambda c=c, pp=pp: PE.matmul(pp[:, 0:32], lhsT=ltri_b[:], rhs=OHB[:, c * 32:(c + 1) * 32], start=(c == 0), stop=True),
                     reads=[('OHB', c), 'ltri_b'], writes=[('ppf', c % 2)])
                S.op('dve', lambda pp=pp: V.tensor_tensor(out=base[:], in0=pp[:, 0:32], in1=pstart[:], op=ALU.add), reads=[('ppf', c % 2), 'pstart'], writes=['base'])
                for k in range(2):
                    S.op('dve', lambda k=k: V.tensor_tensor(out=tmp[:, 0:32], in0=OH12[:, c * 64 + k * 32: c * 64 + (k + 1) * 32], in1=base[:], op=ALU.mult),
                         reads=[('OH12', c), 'base'], writes=['tmp'])
                    S.op('dve', lambda k=k: V.tensor_reduce(out=dstf[:, c * 2 + k: c * 2 + k + 1], in_=tmp[:, 0:32], axis=AX.X, op=ALU.add), reads=['tmp'], writes=['dstf'])
            S.op('dve', lambda: V.tensor_copy(out=DEST[:], in_=dstf[:]), reads=['dstf'], writes=['DEST'])
            for c in range(16):
                b = c % 2
                S.dma('sp', lambda c=c, b=b: nc.sync.dma_start(out=hbx[b][:], in_=h1_d[c * 128:(c + 1) * 128, :]), 'xin%d' % b, reads=[('h1_d', c)], writes=[('hbx', b)])
                S.op('act', lambda b=b: A.copy(out=hbb[b][:], in_=hbx[b][:]), reads=[('hbx', b)], writes=[('hbb', b)])
                for k in range(2):
                    S.dma('pool', lambda c=c, b=b, k=k: G.indirect_dma_start(
                        out=xs_d[:, :], out_offset=bass.IndirectOffsetOnAxis(ap=DEST[:, c * 2 + k: c * 2 + k + 1], axis=0), in_=hbb[b][:], in_offset=None,
                        bounds_check=R_XS, oob_is_err=False), 'sct%d' % b, reads=[('hbb', b), 'DEST'], writes=[('xs_d', c, k)])
            S.barrier()
            if stop == 4:
                S.dma('sp', lambda: nc.sync.dma_start(out=dbg[:, 0:32], in_=GATE[:]), 'dbg')
                S.dma('sp', lambda: nc.sync.dma_start(out=dbg[:, 32:64], in_=dstf[:]), 'dbg')
                S.dma('sp', lambda: nc.sync.dma_start(out=dbg[:, 64:128], in_=be[:]), 'dbg')
                S.dma('sp', lambda: nc.sync.dma_start(out=dbg[:, 128:160], in_=pend[:]), 'dbg')
                S.dma('sp', lambda: nc.sync.dma_start(out=dbg[:, 1024:2048], in_=OH12[:]), 'dbg')
                S.muted = True

        with contextlib.ExitStack() as e7:
            SC("s7")
            NS13, NS2 = 16, 8
            W13r = sb(e7, "W13r", [128, NS13 * 2048], BF16)
            W2r = sb(e7, "W2r", [128, NS2 * 2048], BF16)
            xblk = [sb(e7, "xblk%d" % i, [128, D], BF16) for i in range(2)]
            xT = [sb(e7, "xT%d" % i, [128, 16 * 128], BF16) for i in range(2)]
            sg = sb(e7, "sg", [128, 1024], F32)
            act2 = [sb(e7, "act%d" % i, [128, 1024], BF16) for i in range(2)]
            actT = sb(e7, "actT", [128, 8 * 128], BF16)
            yb = [sb(e7, "yb%d" % i, [128, D], F32) for i in range(2)]
            ptx = [pt(e7, "ptx%d" % i, [128, 1024], BF16) for i in range(2)]
            ph = [pt(e7, "ph%d" % i, [128, 512], F32) for i in range(4)]
            py = [pt(e7, "py%d" % i, [128, 512], F32) for i in range(2)]

            def gather13(n):
                s_ = n % NS13
                S.dma('pool', lambda: G.indirect_dma_start(out=W13r[:, s_ * 2048:(s_ + 1) * 2048], out_offset=None, in_=w13[:, :],
                                                           in_offset=bass.IndirectOffsetOnAxis(ap=IDX13[:, n:n + 1], axis=0),
                                                           bounds_check=R_W13, oob_is_err=False), 'g13_%d' % s_, reads=['IDX13'], writes=[('W13', s_)])

            def gather2(n):
                s_ = n % NS2
                S.dma('pool', lambda: G.indirect_dma_start(out=W2r[:, s_ * 2048:(s_ + 1) * 2048], out_offset=None, in_=w2[:, :],
                                                           in_offset=bass.IndirectOffsetOnAxis(ap=IDX2[:, n:n + 1], axis=0),
                                                           bounds_check=R_W2, oob_is_err=False), 'g2_%d' % s_, reads=['IDX2'], writes=[('W2', s_)])

            def xload(j):
                b = j % 2
                S.dma('sp', lambda: nc.sync.dma_start(out=xblk[b][:], in_=xs_d[j * 128:(j + 1) * 128, :]), 'xin%d' % b, reads=[], writes=[('xblk', b)])

            def xtr(j):
                b = j % 2
                for hf in (1, 0):
                    for k8 in range(8):
                        dc = hf * 8 + k8
                        S.op('pe', lambda dc=dc, k8=k8: PE.transpose(ptx[hf][:, k8 * 128:(k8 + 1) * 128], xblk[b][:, dc * 128:(dc + 1) * 128], ident_b[:]),
                             reads=[('xblk', b), 'ident_b'], writes=[('ptx', hf)])
                    S.op('act', lambda hf=hf: A.copy(out=xT[b][:, hf * 1024:(hf + 1) * 1024], in_=ptx[hf][:]), reads=[('ptx', hf)], writes=[('xT', b, hf)])

            def mm1(j):
                b = j % 2
                for dc in range(16):
                    n = j * 16 + dc
                    s_ = n % NS13
                    for fb in range(4):
                        S.op('pe', lambda dc=dc, fb=fb, s_=s_: PE.matmul(ph[fb][:], lhsT=xT[b][:, dc * 128:(dc + 1) * 128],
                                                                         rhs=W13r[:, s_ * 2048 + fb * 512: s_ * 2048 + (fb + 1) * 512], start=(dc == 0), stop=(dc == 15)),
                             reads=[('xT', b, dc // 8), ('W13', s_)], writes=[('ph', fb)])
                    if n + NS13 < NBLK * 16:
                        gather13(n + NS13)

            def actf(j):
                for hf in range(2):
                    S.op('act', lambda hf=hf: A.activation(out=sg[:, hf * 512:(hf + 1) * 512], in_=ph[hf][:], func=AF.Silu), reads=[('ph', hf)], writes=[('sg', hf)])
                    S.op('dve', lambda hf=hf: V.tensor_tensor(out=act2[j % 2][:, hf * 512:(hf + 1) * 512], in0=sg[:, hf * 512:(hf + 1) * 512], in1=ph[2 + hf][:], op=ALU.mult),
                         reads=[('sg', hf), ('ph', 2 + hf)], writes=[('act', j % 2, hf)])

            def atr(j):
                for fc in range(8):
                    S.op('pe', lambda fc=fc: PE.transpose(ptx[0][:, fc * 128:(fc + 1) * 128], act2[j % 2][:, fc * 128:(fc + 1) * 128], ident_b[:]),
                         reads=[('act', j % 2, fc // 4), 'ident_b'], writes=[('ptx', 0)])
                S.op('act', lambda: A.copy(out=actT[:], in_=ptx[0][:]), reads=[('ptx', 0)], writes=['actT'])

            def mm2(j):
                b = j % 2
                for hf in range(2):
                    for fc in range(8):
                        n = j * 8 + fc
                        s_ = n % NS2
                        for fb in range(2):
                            S.op('pe', lambda fc=fc, fb=fb, s_=s_, hf=hf: PE.matmul(py[fb][:], lhsT=actT[:, fc * 128:(fc + 1) * 128],
                                                                                    rhs=W2r[:, s_ * 2048 + (hf * 2 + fb) * 512: s_ * 2048 + (hf * 2 + fb + 1) * 512],
                                                                                    start=(fc == 0), stop=(fc == 7)),
                                 reads=['actT', ('W2', s_)], writes=[('py', fb)])
                        if hf == 1 and n + NS2 < NBLK * 8:
                            gather2(n + NS2)
                    S.op('act', lambda hf=hf: A.copy(out=yb[b][:, hf * 1024: hf * 1024 + 512], in_=py[0][:]), reads=[('py', 0)], writes=[('yb', b, hf)])
                    S.op('dve', lambda hf=hf: V.tensor_copy(out=yb[b][:, hf * 1024 + 512: hf * 1024 + 1024], in_=py[1][:]), reads=[('py', 1)], writes=[('yb', b, hf)])
                S.dma('sp', lambda: nc.sync.dma_start(out=ys_d[j * 128:(j + 1) * 128, :], in_=yb[b][:]), 'yst%d' % b, reads=[('yb', b, 0), ('yb', b, 1)], writes=[('ys_d', j)])

            for n in range(NS13):
                gather13(n)
            for n in range(NS2):
                gather2(n)
            xload(0)
            xload(1)
            xtr(0)
            for j in range(NBLK):
                mm1(j)
                actf(j)
                if j >= 1:
                    atr(j - 1)
                if j + 1 < NBLK:
                    xtr(j + 1)
                if j + 2 < NBLK:
                    xload(j + 2)
                if j >= 1:
                    mm2(j - 1)
            atr(NBLK - 1)
            mm2(NBLK - 1)
            S.barrier()
            if stop == 7:
                S.muted = True

        with contextlib.ExitStack() as e8:
            SC("s8")
            gb2 = sb(e8, "gb2", [128, 2 * D], F32)
            hx = [sb(e8, "hx%d" % i, [128, D], F32) for i in range(4)]
            y1 = [sb(e8, "y1_%d" % i, [128, D], F32) for i in range(4)]
            y2 = [sb(e8, "y2_%d" % i, [128, D], F32) for i in range(4)]
            S.dma('sp', lambda: nc.sync.dma_start(out=gb2[:], in_=gb_d[:, 4 * D:6 * D]), 'cst', writes=['gb2'])
            for c in range(16):
                b = c % 4
                x_ = hx[b]
                S.dma('sp', lambda: nc.sync.dma_start(out=x_[:], in_=h1_d[c * 128:(c + 1) * 128, :]), 'hxl%d' % b, reads=[('h1_d', c)], writes=[('hx', b)])
                for k, yt in enumerate((y1[b], y2[b])):
                    S.dma('pool', lambda k=k, yt=yt: G.indirect_dma_start(out=yt[:], out_offset=None, in_=ys_d[:, :],
                                                                          in_offset=bass.IndirectOffsetOnAxis(ap=DEST[:, c * 2 + k: c * 2 + k + 1], axis=0),
                                                                          bounds_check=R_XS, oob_is_err=False), 'yg%d%d' % (k, b),
                          reads=['DEST'], writes=[('y', k, b)])
                S.op('act', lambda: A.activation(out=x_[:], in_=x_[:], func=AF.Identity, scale=ALPHA), reads=[('hx', b)], writes=[('hx', b)])
                for k, yt in enumerate((y1[b], y2[b])):
                    S.op('dve', lambda k=k, yt=yt: V.scalar_tensor_tensor(out=x_[:], in0=yt[:], scalar=GATE[:, c * 2 + k: c * 2 + k + 1], in1=x_[:], op0=ALU.mult, op1=ALU.add),
                         reads=[('y', k, b), ('hx', b), ('GATE', c)], writes=[('hx', b)])
                mean, rstd, rk = ln_stats(x_, ('hx', b), gi)
                gi += 1
                normalize(x_[:], x_[:], mean, rstd, [('hx', b)] + rk, [('hx', b)])
                S.op('pool', lambda: G.tensor_tensor(out=x_[:], in0=x_[:], in1=gb2[:, 0:D], op=ALU.mult), reads=[('hx', b), 'gb2'], writes=[('hx', b)])
                S.op('dve', lambda: V.tensor_tensor(out=x_[:], in0=x_[:], in1=gb2[:, D:2 * D], op=ALU.add), reads=[('hx', b), 'gb2'], writes=[('hx', b)])
                S.dma('sp', lambda: nc.sync.dma_start(out=out[c * 128:(c + 1) * 128, :], in_=x_[:]), 'ost%d' % b, reads=[('hx', b)], writes=[('out', c)])
            S.muted = False
            S.barrier()
            scope.close()
    return nc


_NC = None


def _consts(half):
    ident = np.eye(128, dtype=np.float32)
    tp = np.arange(128)
    ltri = (tp[:, None] < tp[None, :]).astype(np.float32)
    utri = (tp[:, None] <= tp[None, :]).astype(np.float32)
    masks = np.zeros((128, 18, 512), np.float32)
    k = np.arange(128)[:, None]
    q = np.arange(512)[None, :]
    for s_ in range(2):
        off = TILES[half][s_] % 2
        for i in range(9):
            masks[:, s_ * 9 + i, :] = (128 * i + k <= 16 + 512 * off + q)
    return ident, ltri, utri, masks.reshape(128, 18 * 512)


def half_off(half):
    return half


def kernel(x, meta, ln_in_g, ln_in_b, w_in, b_f, pool_w, pool_scale, w_out, ln_mix_g, ln_mix_b,
           w_router_g, b_router_g, w_router_e, b_router_e, w13, w2, ln_ffn_g, ln_ffn_b):
    global _NC
    import os
    stop = float(os.environ.get("KSTOP", "99"))
    ncores = int(os.environ.get("KCORES", "8"))
    f = np.float32
    x = np.asarray(x, f)
    meta = np.asarray(meta, f)
    if _NC is None:
        _NC = build(stop)
    nc = _NC
    w_in0 = np.ascontiguousarray(np.asarray(w_in, f)[0])
    w_out0 = np.ascontiguousarray(np.asarray(w_out, f)[0])
    pool_w0 = np.ascontiguousarray(np.asarray(pool_w, f)[0].reshape(4 * 256, 256))
    w13_0 = np.ascontiguousarray(np.asarray(w13, f)[0].reshape(32 * 2048, 2048))
    w2_0 = np.ascontiguousarray(np.asarray(w2, f)[0].reshape(32 * 1024, 2048))
    gT = np.ascontiguousarray(np.asarray(ln_in_g, f).reshape(16, 128).T)
    bT = np.ascontiguousarray(np.asarray(ln_in_b, f).reshape(16, 128).T)
    rep = lambda v: np.broadcast_to(np.asarray(v, f).reshape(1, -1), (128, np.asarray(v).size))
    gb = np.ascontiguousarray(np.concatenate([rep(ln_in_g), rep(ln_in_b), rep(ln_mix_g[0]), rep(ln_mix_b[0]),
                                              rep(ln_ffn_g[0]), rep(ln_ffn_b[0])], axis=1))
    pscale = np.ascontiguousarray(np.asarray(pool_scale, f)[0].reshape(8, 128).T)
    bf_b = np.ascontiguousarray(rep(b_f[0]))
    brt_b = np.ascontiguousarray(np.concatenate([rep(b_router_g[0]), rep(b_router_e[0])], axis=1))
    wr_full = np.concatenate([np.asarray(w_router_g, f)[0], np.asarray(w_router_e, f)[0]], axis=1)
    wr = np.ascontiguousarray(wr_full.reshape(16, 128, 36).transpose(1, 0, 2).reshape(128, 16 * 36))
    iota = np.ascontiguousarray((np.arange(128, dtype=f)[:, None] + 128.0 * np.arange(24, dtype=f)[None, :]))
    jv = np.ascontiguousarray(np.broadcast_to((128.0 * np.arange(NBLK, dtype=f))[None, :, None], (128, NBLK, 32)).reshape(128, NBLK * 32))
    in_maps = []
    for c in range(8):
        b, half = c // 2, c % 2
        xfull = np.zeros((LP, D), f)
        xfull[0:16] = meta
        xfull[16:16 + 4096] = x[b]
        xown = np.zeros((4, 640, D), f)
        sel = np.zeros((LP, 16), f)
        for p, T in enumerate(TILES[half]):
            ps = 16 + 512 * T
            lo = ps - 128
            if lo < 0:
                xown[p, -lo:] = xfull[0:ps + 512]
            else:
                xown[p] = xfull[lo:ps + 512]
            for j in range(4):
                sel[ps + 128 * j + 127, 4 * p + j] = 1.0
        sel_l = np.ascontiguousarray(sel.reshape(NCH, 128, 16).transpose(1, 0, 2).reshape(128, NCH * 16))
        ident, ltri, utri, masks = _consts(half)
        in_maps.append(dict(xfull=xfull, xown=xown.reshape(4 * 640, D), w_in=w_in0, w_out=w_out0, pool_w=pool_w0, w13=w13_0, w2=w2_0,
                            gT=gT, bT=bT, gb=gb, pscale=pscale, bf_b=bf_b, brt_b=brt_b, wr=wr, ident=ident, ltri=ltri, utri=utri,
                            masks=masks, sel=sel_l, iota=iota, jv=jv))
    if stop < 7:
        for m in in_maps:
            m["w13"] = w13_0[:128]
            m["w2"] = w2_0[:128]
    if os.environ.get("KTRACE"):
        res = run_bass_kernel_spmd(nc, in_maps[:ncores], core_ids=list(range(ncores)), trace=True)
        print("EXEC_NS", res.exec_time_ns, "mean", res.mean_exec_time_ns, "maxcore", res.max_exec_time_core_id)
        for k_, v_ in (res.per_core_scope_times or {}).items():
            print("SCOPE", k_, {c_: round(t_ / 1000) for c_, t_ in v_.items()})
    else:
        res = run_bass_kernel_spmd(nc, in_maps[:ncores], core_ids=list(range(ncores)))
    if stop < 99:
        global _DBG
        _DBG = (res, in_maps)
        return None
    outp = np.zeros((4, 4096, D), f)
    for c in range(8):
        b, half = c // 2, c % 2
        o = res.results[c]["out"]
        for p, T in enumerate(TILES[half]):
            outp[b, T * 512:(T + 1) * 512] = o[p * 512:(p + 1) * 512]
    return outp
```

```python
import contextlib
import numpy as np
import concourse.bass as bass
import concourse.mybir as mybir
from concourse.bass_utils import run_bass_kernel_spmd

F32 = mybir.dt.float32
BF16 = mybir.dt.bfloat16
I32 = mybir.dt.int32
AF = mybir.ActivationFunctionType
ALU = mybir.AluOpType
AX = mybir.AxisListType

D = 2048
NCH = 33
LP = NCH * 128
NOWN = 2048
DIN = 4104
ALPHA = 2.0 ** 0.25
EPS = 1e-5
SCALE = 128.0 ** -0.5
NBLK = 64
TILES = {0: [0, 3, 4, 7], 1: [1, 2, 5, 6]}


class Sched:
    def __init__(self, nc, es):
        self.nc = nc
        self.eng = {'pe': nc.tensor, 'dve': nc.vector, 'act': nc.scalar,
                    'pool': nc.gpsimd, 'sp': nc.sync}
        self.sem = {k: es.enter_context(nc.semaphore("s_" + k)) for k in self.eng}
        self.seq = {k: 0 for k in self.eng}
        self.known = {k: {} for k in self.eng}
        self.lastw = {}
        self.readers = {}
        self.dsem = {}
        self.dcons = {}
        self.es = es
        self.muted = False

    def _dma_sem(self, name):
        if name not in self.dsem:
            self.dsem[name] = [self.es.enter_context(self.nc.semaphore("d_" + name)), 0]
        return self.dsem[name]

    def _wait(self, e, tok):
        kind, name, val = tok
        if kind == 'eng' and name == e and e in ('pe', 'sp'):
            return
        k = (kind, name)
        if self.known[e].get(k, 0) >= val:
            return
        self.known[e][k] = val
        sem = self.sem[name] if kind == 'eng' else self.dsem[name][0]
        if kind == 'dma':
            self.dcons[name] = max(self.dcons.get(name, 0), val)
        self.eng[e].wait_ge(sem, val)

    def deps(self, e, reads, writes):
        toks = []
        for r in reads:
            if r in self.lastw:
                toks.append(self.lastw[r])
        for w in writes:
            if w in self.lastw:
                toks.append(self.lastw[w])
            toks.extend(self.readers.get(w, ()))
        for t in toks:
            self._wait(e, t)

    def commit(self, tok, reads, writes):
        for r in reads:
            self.readers.setdefault(r, []).append(tok)
        for w in writes:
            self.lastw[w] = tok
            self.readers[w] = []

    def op(self, e, fn, reads=(), writes=()):
        if self.muted:
            return None
        self.deps(e, reads, writes)
        ins = fn()
        self.seq[e] += 1
        ins.then_inc(self.sem[e], 1)
        tok = ('eng', e, self.seq[e])
        self.commit(tok, reads, writes)
        return tok

    def dma(self, e, fn, sem, reads=(), writes=()):
        if self.muted:
            return None
        self.deps(e, reads, writes)
        s = self._dma_sem(sem)
        if self.dcons.get(sem, 0) > 0:
            self._wait(e, ('dma', sem, self.dcons[sem]))
        ins = fn()
        s[1] += 16
        ins.then_inc(s[0], 16)
        tok = ('dma', sem, s[1])
        self.commit(tok, reads, writes)
        return tok

    def regroup(self, tok, keys):
        if self.muted:
            return
        for k in keys:
            self.lastw[k] = tok

    def wait_all(self, e):
        for name, (s, c) in self.dsem.items():
            if c:
                self._wait(e, ('dma', name, c))
        for k in self.eng:
            if self.seq[k] and k != e:
                self._wait(e, ('eng', k, self.seq[k]))

    def barrier(self):
        for e in self.eng:
            self.wait_all(e)


def build(stop=99):
    DEBUG = stop < 99
    nc = bass.Bass("TRN2", target_bir_lowering=False)

    def din(name, shape, dt=F32):
        return nc.dram_tensor(name, shape, dt, kind="ExternalInput").ap()

    def dscr(name, shape, dt):
        return nc.dram_tensor(name, shape, dt, kind="ExternalOutput" if DEBUG else "Internal").ap()

    xfull = din("xfull", [LP, D])
    xown = din("xown", [4 * 640, D])
    w_in = din("w_in", [D, DIN])
    w_out = din("w_out", [D, D])
    pool_w = din("pool_w", [4 * 256, 256])
    w13 = din("w13", [32 * 2048 if stop >= 7 else 128, 2048])
    w2 = din("w2", [32 * 1024 if stop >= 7 else 128, 2048])
    dbg = nc.dram_tensor("dbg", [128, 4096], F32, kind="ExternalOutput").ap() if DEBUG else None
    dbgb = nc.dram_tensor("dbgb", [128, 16 * 640], BF16, kind="ExternalOutput").ap() if DEBUG else None
    gT = din("gT", [128, 16])
    bT = din("bT", [128, 16])
    gb_d = din("gb", [128, 6 * D])
    pscale = din("pscale", [128, 8])
    bf_b = din("bf_b", [128, 8])
    brt_b = din("brt_b", [128, 36])
    wr = din("wr", [128, 16 * 36])
    ident = din("ident", [128, 128])
    ltri = din("ltri", [128, 128])
    utri = din("utri", [128, 128])
    masks = din("masks", [128, 18 * 512])
    sel = din("sel", [128, NCH * 16])
    iota = din("iota", [128, 24])
    jv = din("jv", [128, NBLK * 32])
    out = nc.dram_tensor("out", [NOWN, D], F32, kind="ExternalOutput").ap()
    kT_d = dscr("kT_d", [8 * 128, LP], BF16)
    v_d = dscr("v_d", [8 * 128, NCH * 128], BF16)
    h1_d = dscr("h1_d", [NOWN, D], F32)
    xs_d = dscr("xs_d", [NBLK * 128, D], BF16)
    ys_d = dscr("ys_d", [NBLK * 128, D], F32)
    q_d = dscr("q_d", [8 * 128, NOWN], BF16)
    mix_d = dscr("mix_d", [16 * 128, NOWN], BF16)
    mix_dv = mix_d.rearrange("(c d) t -> d c t", c=16)
    v_dv = v_d.rearrange("(h t) (g d) -> t h g d", h=8, g=NCH)

    with contextlib.ExitStack() as es:
        S = Sched(nc, es)

        def sb(st, n, sh, dt):
            return st.enter_context(nc.sbuf_tensor(n, sh, dt))

        def pt(st, n, sh, dt):
            return st.enter_context(nc.psum_tensor(n, sh, dt))

        V, A, G, PE = nc.vector, nc.scalar, nc.gpsimd, nc.tensor
        R_XS = G.to_reg(NBLK * 128 - 1)
        R_W13 = G.to_reg(32 * 2048 - 1)
        R_W2 = G.to_reg(32 * 1024 - 1)

        ident_f = sb(es, "ident_f", [128, 128], F32)
        ident_b = sb(es, "ident_b", [128, 128], BF16)
        ones_f = sb(es, "ones_f", [128, 128], F32)
        ones_b = sb(es, "ones_b", [128, 128], BF16)
        ltri_b = sb(es, "ltri_b", [128, 128], BF16)
        ltri_f = sb(es, "ltri_f", [128, 128], F32)
        utri_f = sb(es, "utri_f", [128, 128], F32)
        gT_s = sb(es, "gT_s", [128, 16], F32)
        bT_s = sb(es, "bT_s", [128, 16], F32)
        iota_s = sb(es, "iota_s", [128, 24], F32)
        stt = sb(es, "stt", [128, 2 * 24], F32)
        mvt = sb(es, "mvt", [128, 2 * 4], F32)
        LG = sb(es, "LG", [128, 16 * 36], F32)
        GATE = sb(es, "GATE", [128, 16 * 2], F32)
        DEST = sb(es, "DEST", [128, 16 * 2], I32)
        OHB = sb(es, "OHB", [128, 16 * 32], BF16)
        OH12 = sb(es, "OH12", [128, 16 * 64], F32)
        IDX13 = sb(es, "IDX13", [128, NBLK * 16], I32)
        IDX2 = sb(es, "IDX2", [128, NBLK * 8], I32)

        def cload(dst, src, key):
            S.dma('sp', lambda: nc.sync.dma_start(out=dst, in_=src), 'cst', writes=[key])
        cload(ident_f[:], ident[:, :], 'ident_f')
        cload(ltri_f[:], ltri[:, :], 'ltri_f')
        cload(utri_f[:], utri[:, :], 'utri_f')
        cload(gT_s[:], gT[:, :], 'gT')
        cload(bT_s[:], bT[:, :], 'bT')
        tk = None
        S.dma('sp', lambda: nc.sync.dma_start(out=iota_s[:], in_=iota[:, :]), 'cst', writes=['iota'])
        S.regroup(S.lastw.get('iota'), ['ident_f', 'ltri_f', 'utri_f', 'gT', 'bT', 'iota'])
        S.op('dve', lambda: V.tensor_copy(out=ident_b[:], in_=ident_f[:]), reads=['ident_f'], writes=['ident_b'])
        S.op('dve', lambda: V.tensor_copy(out=ltri_b[:], in_=ltri_f[:]), reads=['ltri_f'], writes=['ltri_b'])
        S.op('dve', lambda: V.memset(ones_f[:], 1.0), writes=['ones_f'])
        S.op('dve', lambda: V.memset(ones_b[:], 1.0), writes=['ones_b'])
        if stop == 0.1:
            S.muted = True

        def ln_stats(src, kin, i):
            b = i % 2
            st = stt[:, b * 24:(b + 1) * 24]
            for q in range(4):
                S.op('dve', lambda q=q: V.bn_stats(out=st[:, q * 6:(q + 1) * 6], in_=src[:, q * 512:(q + 1) * 512]),
                     reads=[kin], writes=[('st', b, q)])
            S.op('dve', lambda: V.bn_aggr(out=mvt[:, b * 4:b * 4 + 2], in_=st),
                 reads=[('st', b, q) for q in range(4)], writes=[('mv', b)])
            S.op('act', lambda: A.activation(out=mvt[:, b * 4 + 2:b * 4 + 3], in_=mvt[:, b * 4 + 1:b * 4 + 2],
                                             func=AF.Sqrt, bias=EPS), reads=[('mv', b)], writes=[('sd', b)])
            S.op('dve', lambda: V.reciprocal(out=mvt[:, b * 4 + 3:b * 4 + 4], in_=mvt[:, b * 4 + 2:b * 4 + 3]),
                 reads=[('sd', b)], writes=[('rs', b)])
            return mvt[:, b * 4:b * 4 + 1], mvt[:, b * 4 + 3:b * 4 + 4], [('mv', b), ('rs', b)]

        def normalize(dst, src, mean, rstd, reads, writes):
            S.op('dve', lambda: V.tensor_scalar(out=dst, in0=src, scalar1=mean, scalar2=rstd,
                                                op0=ALU.subtract, op1=ALU.mult), reads=reads, writes=writes)

        with contextlib.ExitStack() as esA:
            c_all = sb(esA, "c_all", [128, NCH * 8], F32)
            CJ = sb(esA, "CJ", [128, 128], F32)

            with contextlib.ExitStack() as e1:
                Wb = sb(e1, "Wb", [128, 16 * 2056], BF16)
                xin = [sb(e1, "xin%d" % i, [128, D], F32) for i in range(2)]
                hT = [sb(e1, "hT%d" % i, [128, 16 * 640], BF16) for i in range(2)]
                psc = sb(e1, "psc", [128, 8], F32)
                e1a = contextlib.ExitStack()
                kst = [sb(e1a, "kst%d" % i, [128, 512], BF16) for i in range(2)]
                vst = [sb(e1a, "vst%d" % i, [128, 1024], BF16) for i in range(2)]
                lf_all = sb(e1a, "lf_all", [128, NCH * 8], F32)
                carry = sb(e1a, "carry", [128, NCH * 8], F32)
                tot_s = sb(e1a, "tot_s", [128, NCH * 8], F32)
                zt = sb(e1a, "zt", [128, 16], F32)
                bf_s = sb(e1a, "bf_s", [128, 8], F32)
                sel_s = sb(e1a, "sel_s", [128, NCH * 16], F32)
                Rt = sb(e1a, "Rt", [128, 2 * 128], F32)
                pst = pt(e1, "pst", [128, 1024], F32)
                pa = [pt(e1, "pa%d" % i, [128, 512], F32) for i in range(6)]

                cload(bf_s[:], bf_b[:, :], 'bf_s')
                cload(sel_s[:], sel[:, :], 'sel_s')
                cload(psc[:], pscale[:, :], 'psc')
                S.regroup(S.lastw.get('psc'), ['bf_s', 'sel_s', 'psc'])

                def load_w(cols):
                    keys = []
                    t = None
                    for dc in range(16):
                        for (c0, n, d0) in cols:
                            t = S.dma('pool', lambda dc=dc, c0=c0, n=n, d0=d0: G.dma_start(
                                out=Wb[:, dc * 2056 + d0: dc * 2056 + d0 + n],
                                in_=w_in[dc * 128:(dc + 1) * 128, c0:c0 + n]), 'wld', writes=['Wb'])
                    S.regroup(t, ['Wb'])

                def front(src_ap, i, hbuf, hstride, hcol):
                    b = i % 2
                    S.dma('sp', lambda: nc.sync.dma_start(out=xin[b][:], in_=src_ap), 'xin%d' % b, writes=[('xin', b)])
                    mean, rstd, rk = ln_stats(xin[b], ('xin', b), i)
                    normalize(xin[b][:], xin[b][:], mean, rstd, [('xin', b)] + rk, [('xin', b)])
                    for r4 in range(4):
                        bk = r4 % 2
                        for k4 in range(4):
                            dc = r4 * 4 + k4
                            S.op('pe', lambda dc=dc, bk=bk, k4=k4: PE.transpose(pst[:, bk * 512 + k4 * 128: bk * 512 + (k4 + 1) * 128],
                                                                                xin[b][:, dc * 128:(dc + 1) * 128], ident_f[:]),
                                 reads=[('xin', b), 'ident_f'], writes=[('pst', bk)])
                        for k4 in range(4):
                            dc = r4 * 4 + k4
                            S.op('act', lambda dc=dc, bk=bk, k4=k4: A.activation(out=hbuf[:, dc * hstride + hcol: dc * hstride + hcol + 128],
                                                                                 in_=pst[:, bk * 512 + k4 * 128: bk * 512 + (k4 + 1) * 128], func=AF.Identity,
                                                                                 scale=gT_s[:, dc:dc + 1], bias=bT_s[:, dc:dc + 1]),
                                 reads=[('pst', bk), 'gT', 'bT'], writes=[('hT', id(hbuf), hcol, dc)])
                    if stop == 0.45:
                        S.muted = True

                scope = es.enter_context(contextlib.ExitStack())
                def SC(name):
                    scope.close()
                    scope.enter_context(nc.named_scope(name))
                SC("s1a")
                load_w([(1024, 2056, 0)])
                if stop == 0.2:
                    S.muted = True
                tiles = [(i * 512, 512) for i in range(8)] + [(4096, 128)]
                gi = 0
                for ti, (t0, TW) in enumerate(tiles):
                    hb = hT[ti % 2]
                    for cc in range(TW // 128):
                        g = t0 // 128 + cc
                        front(xfull[g * 128:(g + 1) * 128, :], gi, hb, 640, cc * 128)
                        gi += 1
                        if stop == 0.45:
                            S.muted = True
                        if stop == 0.5:
                            S.dma('sp', lambda: nc.sync.dma_start(out=dbgb[:, :], in_=hb[:]), 'dbg', reads=hkd)
                            S.muted = True
                        hk = ('hT', id(hb), cc * 128)
                        hkd = [('hT', id(hb), cc * 128, dc_) for dc_ in range(16)]
                        for dc in range(16):
                            lhs = hb[:, dc * 640 + cc * 128: dc * 640 + cc * 128 + 128]
                            S.op('pe', lambda dc=dc, lhs=lhs: PE.matmul(pa[0][:], lhsT=lhs, rhs=Wb[:, dc * 2056 + 1024: dc * 2056 + 1536],
                                                                        start=(dc == 0), stop=(dc == 15)), reads=[hkd[dc], 'Wb'], writes=['pv0'])
                            S.op('pe', lambda dc=dc, lhs=lhs: PE.matmul(pa[1][:], lhsT=lhs, rhs=Wb[:, dc * 2056 + 1536: dc * 2056 + 2048],
                                                                        start=(dc == 0), stop=(dc == 15)), reads=[hkd[dc], 'Wb'], writes=['pv1'])
                            S.op('pe', lambda dc=dc, lhs=lhs: PE.matmul(pa[2][:, 0:8], lhsT=lhs, rhs=Wb[:, dc * 2056 + 2048: dc * 2056 + 2056],
                                                                        start=(dc == 0), stop=(dc == 15)), reads=[hkd[dc], 'Wb'], writes=['pf'])
                        vb = g % 2
                        S.op('act', lambda vb=vb: A.copy(out=vst[vb][:, 0:512], in_=pa[0][:]), reads=['pv0'], writes=[('vst', vb, 0)])
                        S.op('dve', lambda vb=vb: V.tensor_copy(out=vst[vb][:, 512:1024], in_=pa[1][:]), reads=['pv1'], writes=[('vst', vb, 1)])
                        S.dma('sp', lambda vb=vb, g=g: nc.sync.dma_start(out=v_dv[:, :, g, :], in_=vst[vb][:].rearrange("t (h d) -> t h d", h=8)),
                              'vst%d' % vb, reads=[('vst', vb, 0), ('vst', vb, 1)], writes=[('v_d', g)])
                        S.op('dve', lambda: V.tensor_tensor(out=zt[:, 0:8], in0=pa[2][:, 0:8], in1=bf_s[:], op=ALU.add),
                             reads=['pf', 'bf_s'], writes=['zt0'])
                        S.op('act', lambda: A.activation(out=zt[:, 8:16], in_=zt[:, 0:8], func=AF.Exp, scale=-1.0), reads=['zt0'], writes=['zt1'])
                        S.op('act', lambda: A.activation(out=zt[:, 0:8], in_=zt[:, 8:16], func=AF.Ln, bias=1.0), reads=['zt1'], writes=['zt0'])
                        S.op('dve', lambda g=g: V.tensor_scalar(out=lf_all[:, g * 8:(g + 1) * 8], in0=zt[:, 0:8], scalar1=-1.0, scalar2=None,
                                                                op0=ALU.mult), reads=['zt0'], writes=['lf'])
                    hks = [('hT', id(hb), cc * 128, dc_) for cc in range(TW // 128) for dc_ in range(16)]
                    for h in range(8):
                        pk = pa[3 + h % 2]
                        for dc in range(16):
                            S.op('pe', lambda dc=dc, h=h, pk=pk: PE.matmul(pk[:, 0:TW], lhsT=Wb[:, dc * 2056 + h * 128: dc * 2056 + h * 128 + 128],
                                                                           rhs=hb[:, dc * 640: dc * 640 + TW], start=(dc == 0), stop=(dc == 15)),
                                 reads=hks + ['Wb'], writes=[('pk', h % 2)])
                        kb = h % 2
                        if h % 2 == 0:
                            S.op('act', lambda pk=pk, kb=kb: A.copy(out=kst[kb][:, 0:TW], in_=pk[:, 0:TW]), reads=[('pk', kb)], writes=[('kst', kb)])
                        else:
                            S.op('dve', lambda pk=pk, kb=kb: V.tensor_copy(out=kst[kb][:, 0:TW], in_=pk[:, 0:TW]), reads=[('pk', kb)], writes=[('kst', kb)])
                        S.dma('sp', lambda h=h, kb=kb: nc.sync.dma_start(out=kT_d[h * 128:(h + 1) * 128, t0:t0 + TW], in_=kst[kb][:, 0:TW]),
                              'kst%d' % kb, reads=[('kst', kb)], writes=[('kT_d', h)])
                    if stop == 0.6:
                        S.muted = True

                if stop == 0.8:
                    S.dma('sp', lambda: nc.sync.dma_start(out=dbg[:, 0:264], in_=lf_all[:]), 'dbg')
                    S.muted = True
                SC("s1c")
                S.op('pe', lambda: PE.matmul(pa[0][:, 0:264], lhsT=ones_f[:], rhs=lf_all[:], start=True, stop=True), reads=['lf', 'ones_f'], writes=['pv0'])
                S.op('pe', lambda: PE.matmul(pa[1][:, 0:264], lhsT=utri_f[:], rhs=lf_all[:], start=True, stop=True), reads=['lf', 'utri_f'], writes=['pv1'])
                S.op('dve', lambda: V.tensor_copy(out=tot_s[:], in_=pa[0][:, 0:264]), reads=['pv0'], writes=['tot'])
                S.op('dve', lambda: V.memset(carry[:, 0:8], 0.0), writes=['carry'])
                for g in range(1, NCH):
                    S.op('dve', lambda g=g: V.tensor_tensor(out=carry[:, g * 8:(g + 1) * 8], in0=carry[:, (g - 1) * 8:g * 8],
                                                            in1=tot_s[:, (g - 1) * 8:g * 8], op=ALU.add), reads=['carry', 'tot'], writes=['carry'])
                S.op('dve', lambda: V.tensor_tensor(out=c_all[:], in0=pa[1][:, 0:264], in1=carry[:], op=ALU.add), reads=['pv1', 'carry'], writes=['c_all'])
                for g in range(NCH):
                    rb = g % 2
                    S.op('dve', lambda g=g, rb=rb: V.tensor_tensor(
                        out=Rt[:, rb * 128:(rb + 1) * 128].rearrange("p (h j) -> p h j", h=8),
                        in0=c_all[:, g * 8:(g + 1) * 8].unsqueeze(2).to_broadcast([128, 8, 16]),
                        in1=sel_s[:, g * 16:(g + 1) * 16].unsqueeze(1).to_broadcast([128, 8, 16]), op=ALU.mult),
                        reads=['c_all', 'sel_s'], writes=[('Rt', rb)])
                    S.op('pe', lambda g=g, rb=rb: PE.matmul(pa[2][:, 0:128], lhsT=ones_f[:], rhs=Rt[:, rb * 128:(rb + 1) * 128],
                                                            start=(g == 0), stop=(g == NCH - 1)), reads=[('Rt', rb), 'ones_f'], writes=['pf'])
                S.op('dve', lambda: V.tensor_copy(out=CJ[:], in_=pa[2][:, 0:128]), reads=['pf'], writes=['CJ'])

                S.barrier()
                if stop == 0.9:
                    S.dma('sp', lambda: nc.sync.dma_start(out=dbg[:, 0:264], in_=c_all[:]), 'dbg')
                    S.dma('sp', lambda: nc.sync.dma_start(out=dbg[:, 512:640], in_=CJ[:]), 'dbg')
                    S.muted = True
                e1a.close()
                SC("s1b")
                U = sb(e1, "U", [128, 8 * 640], F32)
                UA = sb(e1, "UA", [128, 2 * 640], F32)
                UB = sb(e1, "UB", [128, 2 * 640], F32)
                PL = sb(e1, "PL", [128, 8 * 512], BF16)
                PW = sb(e1, "PW", [128, 8 * 256], BF16)
                qst = [sb(e1, "qst%d" % i, [128, 512], BF16) for i in range(2)]
                load_w([(0, 1024, 0), (3080, 1024, 1024)])
                keys = []
                for gq in range(8):
                    S.dma('pool', lambda gq=gq: G.dma_start(out=PW[:, gq * 256:(gq + 1) * 256], in_=pool_w[gq * 128:(gq + 1) * 128, :]), 'pwl', writes=['PW'])
                S.regroup(S.lastw.get('PW'), ['PW'])
                for p in range(4):
                    hb = hT[p % 2]
                    for cc in range(5):
                        front(xown[p * 640 + cc * 128: p * 640 + (cc + 1) * 128, :], gi, hb, 640, cc * 128)
                        gi += 1
                    hks = [('hT', id(hb), cc * 128, dc_) for cc in range(5) for dc_ in range(16)]
                    for h in range(8):
                        pk = pa[h % 2]
                        for dc in range(16):
                            S.op('pe', lambda dc=dc, h=h, pk=pk: PE.matmul(pk[:], lhsT=Wb[:, dc * 2056 + h * 128: dc * 2056 + h * 128 + 128],
                                                                           rhs=hb[:, dc * 640 + 128: dc * 640 + 640], start=(dc == 0), stop=(dc == 15)),
                                 reads=hks + ['Wb'], writes=[('pq', h % 2)])
                        dst = qst[h % 2][:]
                        if h % 2 == 0:
                            S.op('act', lambda pk=pk, dst=dst: A.copy(out=dst, in_=pk[:]), reads=[('pq', 0)], writes=[('qst', 0)])
                        else:
                            S.op('dve', lambda pk=pk, dst=dst: V.tensor_copy(out=dst, in_=pk[:]), reads=[('pq', 1)], writes=[('qst', 1)])
                        S.dma('sp', lambda h=h, dst=dst: nc.sync.dma_start(out=q_d[h * 128:(h + 1) * 128, p * 512:(p + 1) * 512], in_=dst), 'kst%d' % (h % 2),
                              reads=[('qst', h % 2)], writes=[('q_d', h, p)])
                    for ch in range(8):
                        pA, pB = pa[2 + 2 * (ch % 2)], pa[3 + 2 * (ch % 2)]
                        for dc in range(16):
                            lhs = Wb[:, dc * 2056 + 1024 + ch * 128: dc * 2056 + 1024 + ch * 128 + 128]
                            S.op('pe', lambda dc=dc, lhs=lhs, pA=pA: PE.matmul(pA[:], lhsT=lhs, rhs=hb[:, dc * 640 + 128: dc * 640 + 640],
                                                                               start=(dc == 0), stop=(dc == 15)), reads=hks + ['Wb'], writes=[('pu', ch % 2, 0)])
                            S.op('pe', lambda dc=dc, lhs=lhs, pB=pB: PE.matmul(pB[:, 0:16], lhsT=lhs, rhs=hb[:, dc * 640 + 112: dc * 640 + 128],
                                                                               start=(dc == 0), stop=(dc == 15)), reads=hks + ['Wb'], writes=[('pu', ch % 2, 1)])
                        S.op('act', lambda ch=ch, pA=pA: A.copy(out=U[:, ch * 640 + 128:(ch + 1) * 640], in_=pA[:]), reads=[('pu', ch % 2, 0)], writes=[('U', ch)])
                        S.op('act', lambda ch=ch, pB=pB: A.copy(out=U[:, ch * 640 + 112:ch * 640 + 128], in_=pB[:, 0:16]), reads=[('pu', ch % 2, 1)], writes=[('U', ch)])
                    for ch in range(8):
                        gg = ch // 2
                        eng, E = ('dve', V) if ch % 2 == 0 else ('pool', G)
                        sl = ch % 2
                        u = U[:, ch * 640:(ch + 1) * 640]
                        a = UA[:, sl * 640:(sl + 1) * 640]
                        bq = UB[:, sl * 640:(sl + 1) * 640]
                        cur, curk = u, ('U', ch)
                        lo, st = 112, 1
                        bufs = [(a, ('UA', sl)), (bq, ('UB', sl))]
                        for step in range(gg + 1):
                            dstb, dk = bufs[step % 2]
                            lo2 = lo + st
                            S.op(eng, lambda E=E, dstb=dstb, cur=cur, lo2=lo2, st=st: E.tensor_tensor(
                                out=dstb[:, lo2:640], in0=cur[:, lo2:640], in1=cur[:, lo2 - st:640 - st], op=ALU.add),
                                reads=[curk], writes=[dk])
                            cur, curk, lo, st = dstb, dk, lo2, st * 2
                        w = float(2 ** (gg + 1))
                        S.op('dve', lambda cur=cur, u=u, ch=ch, w=w: V.scalar_tensor_tensor(
                            out=PL[:, ch * 512:(ch + 1) * 512], in0=cur[:, 128:640], scalar=1.0 / w, in1=u[:, 128:640],
                            op0=ALU.mult, op1=ALU.subtract), reads=[curk, ('U', ch)], writes=[('PL', ch)])
                    for gg in range(4):
                        for dco in range(2):
                            pk = pa[(gg * 2 + dco) % 2]
                            for ci in range(2):
                                S.op('pe', lambda gg=gg, dco=dco, ci=ci, pk=pk: PE.matmul(
                                    pk[:], lhsT=PW[:, (gg * 2 + ci) * 256 + dco * 128:(gg * 2 + ci) * 256 + dco * 128 + 128],
                                    rhs=PL[:, (gg * 2 + ci) * 512:(gg * 2 + ci + 1) * 512], start=(ci == 0), stop=(ci == 1)),
                                    reads=[('PL', gg * 2), ('PL', gg * 2 + 1), 'PW'], writes=[('pq', (gg * 2 + dco) % 2)])
                            oc = 8 + gg * 2 + dco
                            qb_ = (gg * 2 + dco) % 2
                            S.op('act', lambda pk=pk, gg=gg, dco=dco, qb_=qb_: A.activation(
                                out=qst[qb_][:], in_=pk[:], func=AF.Identity,
                                scale=psc[:, gg * 2 + dco: gg * 2 + dco + 1]), reads=[('pq', qb_), 'psc'], writes=[('qst', qb_)])
                            S.dma('sp', lambda oc=oc, qb_=qb_: nc.sync.dma_start(out=mix_d[oc * 128:(oc + 1) * 128, p * 512:(p + 1) * 512], in_=qst[qb_][:]),
                                  'kst%d' % qb_, reads=[('qst', qb_)], writes=[('mix_d', oc, p)])
                S.barrier()
                if stop == 1:
                    S.dma('sp', lambda: nc.sync.dma_start(out=dbg[:, 0:264], in_=c_all[:]), 'dbg')
                    S.dma('sp', lambda: nc.sync.dma_start(out=dbg[:, 512:640], in_=CJ[:]), 'dbg')
                    S.muted = True

            with contextlib.ExitStack() as e2:
                SC("s2")
                kTb = [sb(e2, "kTb%d" % i, [128, LP], BF16) for i in range(2)]
                Vb = [sb(e2, "Vb%d" % i, [128, NCH * 128], BF16) for i in range(2)]
                PT = [sb(e2, "PT%d" % i, [128, 512], BF16) for i in range(6)]
                MKf = sb(e2, "MKf", [128, 512], F32)
                MK = sb(e2, "MK", [128, 18 * 512], BF16)
                BT = sb(e2, "BT", [128, 8 * NCH * 16], F32)
                Qb = [sb(e2, "Qb%d" % i, [128, NOWN], BF16) for i in range(2)]
                ost = [sb(e2, "ost%d" % i, [128, 512], BF16) for i in range(2)]
                rden = sb(e2, "rden", [128, 512], F32)
                SL = [sb(e2, "SL%d" % i, [128, 512], F32) for i in range(4)]
                ps_s = [pt(e2, "ps_s%d" % i, [128, 512], F32) for i in range(4)]
                ps_o = [pt(e2, "ps_o%d" % i, [128, 512], F32) for i in range(2)]
                ps_d = [pt(e2, "ps_d%d" % i, [128, 512], F32) for i in range(2)]
                for h in range(8):
                    S.op('dve', lambda h=h: V.tensor_tensor(
                        out=BT[:, h * NCH * 16:(h + 1) * NCH * 16].rearrange("p (g j) -> p g j", g=NCH),
                        in0=CJ[:, h * 16:(h + 1) * 16].unsqueeze(1).to_broadcast([128, NCH, 16]),
                        in1=c_all[:].rearrange("p (g h) -> p g h", h=8)[:, :, h:h + 1].to_broadcast([128, NCH, 16]),
                        op=ALU.subtract), reads=['CJ', 'c_all'], writes=['BT'])
                S.op('dve', lambda: V.tensor_scalar(out=BT[:], in0=BT[:], scalar1=0.0, scalar2=None, op0=ALU.min), reads=['BT'], writes=['BT'])
                for i in range(18):
                    S.dma('sp', lambda i=i: nc.sync.dma_start(out=MKf[:], in_=masks[:, i * 512:(i + 1) * 512]), 'mk', writes=['MKf'])
                    S.op('dve', lambda i=i: V.tensor_copy(out=MK[:, i * 512:(i + 1) * 512], in_=MKf[:]), reads=['MKf'], writes=['MK'])
                def load_head(h):
                    hb_ = h % 2
                    S.dma('sp', lambda: nc.sync.dma_start(out=kTb[hb_][:], in_=kT_d[h * 128:(h + 1) * 128, :]), 'kl%d' % hb_,
                          reads=[('kT_d', h)], writes=[('kTb', hb_)])
                    S.dma('sp', lambda: nc.sync.dma_start(out=Vb[hb_][:], in_=v_d[h * 128:(h + 1) * 128, :]), 'vl%d' % hb_,
                          reads=[('v_d', g) for g in range(NCH)], writes=[('Vb', hb_)])
                    S.dma('sp', lambda: nc.sync.dma_start(out=Qb[hb_][:], in_=q_d[h * 128:(h + 1) * 128, :]), 'ql%d' % hb_,
                          reads=[('q_d', h, p) for p in range(4)], writes=[('Qb', hb_)])

                its = [(h, p, kc) for h in range(8) for p in range(4) for kc in range(8 * p + 9)]

                def s_step(i):
                    h, p, kc = its[i]
                    hb_, sbk = h % 2, i % 4
                    S.op('pe', lambda: PE.matmul(ps_s[sbk][:], lhsT=kTb[hb_][:, kc * 128:(kc + 1) * 128], rhs=Qb[hb_][:, p * 512:(p + 1) * 512],
                                                 start=True, stop=True), reads=[('kTb', hb_), ('Qb', hb_)], writes=[('ps_s', sbk)])

                def e_step(i):
                    h, p, kc = its[i]
                    sbk, pb = i % 4, i % 6
                    bcol = (h * NCH + kc) * 16 + 4 * p
                    S.op('dve', lambda: V.scalar_tensor_tensor(
                        out=SL[sbk][:].rearrange("k (j q) -> k j q", j=4), in0=ps_s[sbk][:].rearrange("k (j q) -> k j q", j=4), scalar=SCALE,
                        in1=BT[:, bcol:bcol + 4].unsqueeze(2).to_broadcast([128, 4, 128]), op0=ALU.mult, op1=ALU.add),
                        reads=[('ps_s', sbk), 'BT'], writes=[('SL', sbk)])
                    S.op('act', lambda: A.activation(out=PT[pb][:], in_=SL[sbk][:], func=AF.Exp), reads=[('SL', sbk)], writes=[('PT', pb)])
                    if kc >= 8 * p:
                        mi = (p % 2) * 9 + kc - 8 * p
                        S.op('pool', lambda: G.tensor_tensor(out=PT[pb][:], in0=PT[pb][:], in1=MK[:, mi * 512:(mi + 1) * 512], op=ALU.mult),
                             reads=[('PT', pb), 'MK'], writes=[('PT', pb)])

                def pv_step(i):
                    h, p, kc = its[i]
                    hb_, pb = h % 2, i % 6
                    E_ = 8 * p + 9
                    ob = (h * 4 + p) % 2
                    S.op('pe', lambda: PE.matmul(ps_o[ob][:], lhsT=Vb[hb_][:, kc * 128:(kc + 1) * 128], rhs=PT[pb][:],
                                                 start=(kc == 0), stop=(kc == E_ - 1)), reads=[('Vb', hb_), ('PT', pb)], writes=[('ps_o', ob)])
                    S.op('pe', lambda: PE.matmul(ps_d[ob][:], lhsT=ones_b[:], rhs=PT[pb][:],
                                                 start=(kc == 0), stop=(kc == E_ - 1)), reads=['ones_b', ('PT', pb)], writes=[('ps_d', ob)])
                    if kc == E_ - 1:
                        S.op('dve', lambda: V.reciprocal(out=rden[:], in_=ps_d[ob][:]), reads=[('ps_d', ob)], writes=['rden'])
                        S.op('dve', lambda: V.tensor_tensor(out=ost[ob][:], in0=ps_o[ob][:],
                                                            in1=rden[:], op=ALU.mult), reads=[('ps_o', ob), 'rden'], writes=[('ost', ob)])
                        S.dma('sp', lambda: nc.sync.dma_start(out=mix_d[h * 128:(h + 1) * 128, p * 512:(p + 1) * 512], in_=ost[ob][:]),
                              'ost%d' % ob, reads=[('ost', ob)], writes=[('mix_d', h, p)])

                load_head(0)
                load_head(1)
                LA = 3
                for i in range(LA):
                    s_step(i)
                for i in range(len(its)):
                    h, p, kc = its[i]
                    if i + LA < len(its):
                        s_step(i + LA)
                    e_step(i)
                    pv_step(i)
                    if p == 3 and kc == 8 * p + 8 and h + 2 < 8:
                        load_head(h + 2)
                S.barrier()
                if stop == 2:
                    S.muted = True

            with contextlib.ExitStack() as e3:
                SC("s3")
                WO = sb(e3, "WO", [128, 16 * D], BF16)
                gb = sb(e3, "gb_s", [128, 4 * D], F32)
                xb = [sb(e3, "xb%d" % i, [128, D], F32) for i in range(2)]
                mxt = [sb(e3, "mxt%d" % i, [128, 16 * 512], BF16) for i in range(2)]
                h1T = sb(e3, "h1T", [128, 16 * 128], F32)
                WR = sb(e3, "WR", [128, 16 * 36], F32)
                brt = sb(e3, "brt", [128, 36], F32)
                pm = [pt(e3, "pm%d" % i, [128, 512], F32) for i in range(4)]
                ptr = [pt(e3, "ptr%d" % i, [128, 512], F32) for i in range(2)]
                plg = pt(e3, "plg", [128, 512], F32)
                for dc in range(16):
                    S.dma('pool', lambda dc=dc: G.dma_start(out=WO[:, dc * D:(dc + 1) * D], in_=w_out[dc * 128:(dc + 1) * 128, :]), 'wld', writes=['WO'])
                S.regroup(S.lastw.get('WO'), ['WO'])
                S.dma('sp', lambda: nc.sync.dma_start(out=gb[:], in_=gb_d[:, 0:4 * D]), 'cst', writes=['gb'])
                S.dma('sp', lambda: nc.sync.dma_start(out=WR[:], in_=wr[:, :]), 'cst', writes=['WR'])
                S.dma('sp', lambda: nc.sync.dma_start(out=brt[:], in_=brt_b[:, :]), 'cst', writes=['brt'])
                S.regroup(S.lastw.get('brt'), ['gb', 'WR', 'brt'])
                def s3_load(c):
                    b, p, cc = c % 2, c // 4, c % 4
                    if cc == 0:
                        S.dma('sp', lambda: nc.sync.dma_start(out=mxt[p % 2][:].rearrange("d (c t) -> d c t", c=16), in_=mix_dv[:, :, p * 512:(p + 1) * 512]), 'mxl%d' % (p % 2),
                              reads=[('mix_d', oc_, p) for oc_ in range(16)], writes=[('mxt', p % 2)])
                    S.dma('sp', lambda: nc.sync.dma_start(out=xb[b][:], in_=xown[p * 640 + 128 + cc * 128: p * 640 + 256 + cc * 128, :]), 'xin%d' % b,
                          writes=[('xb', b)])

                def s3_mm(c):
                    p, cc = c // 4, c % 4
                    mx = mxt[p % 2]
                    for fb in range(4):
                        for dc in range(16):
                            S.op('pe', lambda fb=fb, dc=dc: PE.matmul(pm[fb][:], lhsT=mx[:, dc * 512 + cc * 128: dc * 512 + (cc + 1) * 128],
                                                                      rhs=WO[:, dc * D + fb * 512: dc * D + (fb + 1) * 512], start=(dc == 0), stop=(dc == 15)),
                                 reads=[('mxt', p % 2), 'WO'], writes=[('pm', fb)])

                def s3_resid(c, gi):
                    b = c % 2
                    x_ = xb[b]
                    mean, rstd, rk = ln_stats(x_, ('xb', b), gi)
                    normalize(x_[:], x_[:], mean, rstd, [('xb', b)] + rk, [('xb', b)])
                    S.op('pool', lambda: G.tensor_tensor(out=x_[:], in0=x_[:], in1=gb[:, 0:D], op=ALU.mult), reads=[('xb', b), 'gb'], writes=[('xb', b)])
                    S.op('pool', lambda: G.tensor_tensor(out=x_[:], in0=x_[:], in1=gb[:, D:2 * D], op=ALU.add), reads=[('xb', b), 'gb'], writes=[('xb', b)])
                    for fb in range(4):
                        S.op('dve', lambda fb=fb: V.scalar_tensor_tensor(out=x_[:, fb * 512:(fb + 1) * 512], in0=x_[:, fb * 512:(fb + 1) * 512], scalar=ALPHA,
                                                                         in1=pm[fb][:], op0=ALU.mult, op1=ALU.add), reads=[('xb', b), ('pm', fb)], writes=[('xb', b)])

                def s3_post(c, gi):
                    b = c % 2
                    x_ = xb[b]
                    mean, rstd, rk = ln_stats(x_, ('xb', b), gi)
                    normalize(x_[:], x_[:], mean, rstd, [('xb', b)] + rk, [('xb', b)])
                    S.op('pool', lambda: G.tensor_tensor(out=x_[:], in0=x_[:], in1=gb[:, 2 * D:3 * D], op=ALU.mult), reads=[('xb', b), 'gb'], writes=[('xb', b)])
                    S.op('dve', lambda: V.tensor_tensor(out=x_[:], in0=x_[:], in1=gb[:, 3 * D:4 * D], op=ALU.add), reads=[('xb', b), 'gb'], writes=[('xb', b)])
                    S.dma('sp', lambda: nc.sync.dma_start(out=h1_d[c * 128:(c + 1) * 128, :], in_=x_[:]), 'h1s%d' % b, reads=[('xb', b)], writes=[('h1_d', c)])
                    for q4 in range(4):
                        for k4 in range(4):
                            dc = q4 * 4 + k4
                            S.op('pe', lambda dc=dc, q4=q4, k4=k4: PE.transpose(ptr[q4 % 2][:, k4 * 128:(k4 + 1) * 128], x_[:, dc * 128:(dc + 1) * 128], ident_f[:]),
                                 reads=[('xb', b), 'ident_f'], writes=[('ptr', q4 % 2)])
                        if q4 % 2 == 0:
                            S.op('act', lambda q4=q4: A.copy(out=h1T[:, q4 * 512:(q4 + 1) * 512], in_=ptr[q4 % 2][:]), reads=[('ptr', q4 % 2)], writes=['h1T'])
                        else:
                            S.op('dve', lambda q4=q4: V.tensor_copy(out=h1T[:, q4 * 512:(q4 + 1) * 512], in_=ptr[q4 % 2][:]), reads=[('ptr', q4 % 2)], writes=['h1T'])
                    for dc in range(16):
                        S.op('pe', lambda dc=dc: PE.matmul(plg[:, 0:36], lhsT=h1T[:, dc * 128:(dc + 1) * 128], rhs=WR[:, dc * 36:(dc + 1) * 36],
                                                           start=(dc == 0), stop=(dc == 15)), reads=['h1T', 'WR'], writes=['plg'])
                    S.op('dve', lambda: V.tensor_tensor(out=LG[:, c * 36:(c + 1) * 36], in0=plg[:, 0:36], in1=brt[:], op=ALU.add), reads=['plg', 'brt'], writes=[('LG', c)])

                s3_load(0)
                s3_mm(0)
                for c in range(16):
                    if c + 1 < 16:
                        s3_load(c + 1)
                    s3_resid(c, gi)
                    gi += 1
                    if c + 1 < 16:
                        s3_mm(c + 1)
                    s3_post(c, gi)
                    gi += 1
                S.barrier()
        S.barrier()
        if stop == 3:
            S.dma('sp', lambda: nc.sync.dma_start(out=dbg[:, 0:16 * 36], in_=LG[:]), 'dbg')
            S.muted = True

        with contextlib.ExitStack() as e4:
            SC("s4")
            tmp = sb(e4, "tmp", [128, 128], F32)
            ohg = sb(e4, "ohg", [128, 4], F32)
            les = sb(e4, "les", [128, 16], F32)
            oh1 = sb(e4, "oh1", [128, 16], F32)
            sc = sb(e4, "sc", [128, 16], F32)
            cnt = sb(e4, "cnt", [128, 32], F32)
            pad_f = sb(e4, "pad_f", [128, 32], F32)
            pad_i = sb(e4, "pad_i", [128, 32], I32)
            pend = sb(e4, "pend", [128, 32], F32)
            pstart = sb(e4, "pstart", [128, 32], F32)
            base = sb(e4, "base", [128, 32], F32)
            jv_s = sb(e4, "jv_sb", [128, NBLK * 32], F32)
            cmp_ = sb(e4, "cmp", [128, NBLK * 32], F32)
            be = sb(e4, "be", [128, NBLK], F32)
            same = sb(e4, "same", [128, NBLK], F32)
            be13 = sb(e4, "be13", [128, NBLK], F32)
            be2 = sb(e4, "be2", [128, NBLK], F32)
            idf = sb(e4, "idf", [128, NBLK * 16], F32)
            dstf = sb(e4, "dstf", [128, 32], F32)
            hbx = [sb(e4, "hbx%d" % i, [128, D], F32) for i in range(2)]
            hbb = [sb(e4, "hbb%d" % i, [128, D], BF16) for i in range(2)]
            ppf = [pt(e4, "ppf%d" % i, [128, 512], F32) for i in range(2)]
            pcn = pt(e4, "pcn", [128, 512], F32)
            S.dma('sp', lambda: nc.sync.dma_start(out=jv_s[:], in_=jv[:, :]), 'cst', writes=['jv_s'])
            for c in range(16):
                lg = LG[:, c * 36: c * 36 + 4]
                le = LG[:, c * 36 + 4: c * 36 + 36]
                S.op('dve', lambda: V.tensor_reduce(out=sc[:, 0:1], in_=lg, axis=AX.X, op=ALU.max), reads=[('LG', c)], writes=['sc0'])
                S.op('dve', lambda: V.tensor_tensor(out=ohg[:], in0=lg, in1=sc[:, 0:1].to_broadcast([128, 4]), op=ALU.is_equal), reads=[('LG', c), 'sc0'], writes=['ohg'])
                S.op('dve', lambda: V.tensor_scalar(out=sc[:, 1:2], in0=sc[:, 0:1], scalar1=-1.0, scalar2=None, op0=ALU.mult), reads=['sc0'], writes=['sc1'])
                S.op('act', lambda: A.activation(out=tmp[:, 0:4], in_=lg, func=AF.Exp, bias=sc[:, 1:2], accum_out=sc[:, 2:3]), reads=[('LG', c), 'sc1'], writes=['sc2', 'tmp'])
                S.op('dve', lambda: V.reciprocal(out=sc[:, 3:4], in_=sc[:, 2:3]), reads=['sc2'], writes=['sc3'])
                S.op('dve', lambda: V.tensor_scalar(out=les[:, 0:8], in0=le[:, 0:8], scalar1=ohg[:, 0:1], scalar2=None, op0=ALU.mult), reads=[('LG', c), 'ohg'], writes=['les'])
                for g in range(1, 4):
                    S.op('dve', lambda g=g: V.scalar_tensor_tensor(out=les[:, 0:8], in0=le[:, g * 8:(g + 1) * 8], scalar=ohg[:, g:g + 1], in1=les[:, 0:8],
                                                                   op0=ALU.mult, op1=ALU.add), reads=[('LG', c), 'ohg', 'les'], writes=['les'])
                S.op('dve', lambda: V.tensor_reduce(out=sc[:, 4:5], in_=les[:, 0:8], axis=AX.X, op=ALU.max), reads=['les'], writes=['sc4'])
                S.op('dve', lambda: V.tensor_tensor(out=oh1[:, 0:8], in0=les[:, 0:8], in1=sc[:, 4:5].to_broadcast([128, 8]), op=ALU.is_equal), reads=['les', 'sc4'], writes=['oh1'])
                S.op('dve', lambda: V.scalar_tensor_tensor(out=les[:, 8:16], in0=oh1[:, 0:8], scalar=-1e30, in1=les[:, 0:8], op0=ALU.mult, op1=ALU.add),
                     reads=['oh1', 'les'], writes=['les2'])
                S.op('dve', lambda: V.tensor_reduce(out=sc[:, 5:6], in_=les[:, 8:16], axis=AX.X, op=ALU.max), reads=['les2'], writes=['sc5'])
                S.op('dve', lambda: V.tensor_tensor(out=oh1[:, 8:16], in0=les[:, 8:16], in1=sc[:, 5:6].to_broadcast([128, 8]), op=ALU.is_equal), reads=['les2', 'sc5'], writes=['oh2'])
                S.op('dve', lambda: V.tensor_tensor(out=sc[:, 6:7], in0=sc[:, 5:6], in1=sc[:, 4:5], op=ALU.subtract), reads=['sc4', 'sc5'], writes=['sc6'])
                S.op('act', lambda: A.activation(out=sc[:, 7:8], in_=sc[:, 6:7], func=AF.Exp), reads=['sc6'], writes=['sc7'])
                S.op('dve', lambda: V.tensor_scalar(out=sc[:, 8:9], in0=sc[:, 7:8], scalar1=1.0, scalar2=None, op0=ALU.add), reads=['sc7'], writes=['sc8'])
                S.op('dve', lambda: V.reciprocal(out=sc[:, 9:10], in_=sc[:, 8:9]), reads=['sc8'], writes=['sc9'])
                S.op('dve', lambda: V.tensor_tensor(out=GATE[:, c * 2:c * 2 + 1], in0=sc[:, 9:10], in1=sc[:, 3:4], op=ALU.mult), reads=['sc9', 'sc3'], writes=[('GATE', c)])
                S.op('dve', lambda: V.tensor_tensor(out=GATE[:, c * 2 + 1:c * 2 + 2], in0=GATE[:, c * 2:c * 2 + 1], in1=sc[:, 7:8], op=ALU.mult), reads=[('GATE', c), 'sc7'], writes=[('GATE', c)])
                for k in range(2):
                    for g in range(4):
                        S.op('dve', lambda k=k, g=g: V.tensor_scalar(out=OH12[:, c * 64 + k * 32 + g * 8: c * 64 + k * 32 + (g + 1) * 8], in0=oh1[:, k * 8:(k + 1) * 8],
                                                                     scalar1=ohg[:, g:g + 1], scalar2=None, op0=ALU.mult), reads=['oh1', 'oh2', 'ohg'], writes=[('OH12', c)])
                S.op('dve', lambda: V.tensor_tensor(out=OHB[:, c * 32:(c + 1) * 32], in0=OH12[:, c * 64: c * 64 + 32], in1=OH12[:, c * 64 + 32: c * 64 + 64], op=ALU.add),
                     reads=[('OH12', c)], writes=[('OHB', c)])
            for c in range(16):
                S.op('pe', lambda c=c: PE.matmul(pcn[:, 0:32], lhsT=ones_b[:], rhs=OHB[:, c * 32:(c + 1) * 32], start=(c == 0), stop=(c == 15)),
                     reads=[('OHB', c), 'ones_b'], writes=['pcn'])
            S.op('dve', lambda: V.tensor_scalar(out=pad_f[:], in0=pcn[:, 0:32], scalar1=127.0, scalar2=None, op0=ALU.add), reads=['pcn'], writes=['pad_f'])
            S.op('dve', lambda: V.tensor_copy(out=pad_i[:], in_=pad_f[:]), reads=['pad_f'], writes=['pad_i'])
            S.op('dve', lambda: V.tensor_scalar(out=pad_i[:], in0=pad_i[:], scalar1=7, scalar2=7, op0=ALU.arith_shift_right, op1=ALU.logical_shift_left),
                 reads=['pad_i'], writes=['pad_i'])
            S.op('dve', lambda: V.tensor_copy(out=pad_f[:], in_=pad_i[:]), reads=['pad_i'], writes=['pad_f'])
            S.op('dve', lambda: V.tensor_copy(out=pend[:, 0:1], in_=pad_f[:, 0:1]), reads=['pad_f'], writes=['pend'])
            for e in range(1, 32):
                S.op('dve', lambda e=e: V.tensor_tensor(out=pend[:, e:e + 1], in0=pend[:, e - 1:e], in1=pad_f[:, e:e + 1], op=ALU.add), reads=['pend', 'pad_f'], writes=['pend'])
            S.op('dve', lambda: V.tensor_tensor(out=pstart[:], in0=pend[:], in1=pad_f[:], op=ALU.subtract), reads=['pend', 'pad_f'], writes=['pstart'])
            S.op('dve', lambda: V.tensor_tensor(out=cmp_[:].rearrange("p (j e) -> p j e", j=NBLK), in0=jv_s[:].rearrange("p (j e) -> p j e", j=NBLK),
                                                in1=pend[:].unsqueeze(1).to_broadcast([128, NBLK, 32]), op=ALU.is_ge), reads=['jv_s', 'pend'], writes=['cmp'])
            S.op('dve', lambda: V.tensor_reduce(out=be[:], in_=cmp_[:].rearrange("p (j e) -> p j e", j=NBLK), axis=AX.X, op=ALU.add), reads=['cmp'], writes=['be'])
            S.op('dve', lambda: V.tensor_scalar(out=be[:], in0=be[:], scalar1=31.0, scalar2=None, op0=ALU.min), reads=['be'], writes=['be'])
            S.op('dve', lambda: V.memset(same[:, 0:1], 0.0), writes=['same'])
            S.op('dve', lambda: V.tensor_tensor(out=same[:, 1:NBLK], in0=be[:, 1:NBLK], in1=be[:, 0:NBLK - 1], op=ALU.is_equal), reads=['be', 'same'], writes=['same'])
            S.op('dve', lambda: V.tensor_scalar(out=same[:], in0=same[:], scalar1=1.0e6, scalar2=None, op0=ALU.mult), reads=['same'], writes=['same'])
            S.op('dve', lambda: V.scalar_tensor_tensor(out=be13[:], in0=be[:], scalar=2048.0, in1=same[:], op0=ALU.mult, op1=ALU.add), reads=['be', 'same'], writes=['be13'])
            S.op('dve', lambda: V.scalar_tensor_tensor(out=be2[:], in0=be[:], scalar=1024.0, in1=same[:], op0=ALU.mult, op1=ALU.add), reads=['be', 'same'], writes=['be2'])
            S.op('dve', lambda: V.tensor_tensor(out=idf[:].rearrange("p (j c) -> p j c", j=NBLK), in0=be13[:].unsqueeze(2).to_broadcast([128, NBLK, 16]),
                                                in1=iota_s[:, 0:16].unsqueeze(1).to_broadcast([128, NBLK, 16]), op=ALU.add),
                 reads=['be13', 'iota'], writes=['idf'])
            S.op('dve', lambda: V.tensor_copy(out=IDX13[:], in_=idf[:]), reads=['idf'], writes=['IDX13'])
            S.op('dve', lambda: V.tensor_tensor(out=idf[:, 0:NBLK * 8].rearrange("p (j c) -> p j c", j=NBLK), in0=be2[:].unsqueeze(2).to_broadcast([128, NBLK, 8]),
                                                in1=iota_s[:, 0:8].unsqueeze(1).to_broadcast([128, NBLK, 8]), op=ALU.add),
                 reads=['be2', 'iota', 'IDX13'], writes=['idf'])
            S.op('dve', lambda: V.tensor_copy(out=IDX2[:], in_=idf[:, 0:NBLK * 8]), reads=['idf'], writes=['IDX2'])
            for c in range(16):
                pp = ppf[c % 2]
                for c2 in range(c):
                    S.op('pe', lambda c2=c2, pp=pp: PE.matmul(pp[:, 0:32], lhsT=ones_b[:], rhs=OHB[:, c2 * 32:(c2 + 1) * 32], start=(c2 == 0), stop=False),
                         reads=[('OHB', c2), 'ones_b'], writes=[('ppf', c % 2)])
                S.op('pe', lambda c=c, pp=pp: PE.matmul(pp[:, 0:32], lhsT=ltri_b[:], rhs=OHB[:, c * 32:(c + 1) * 32], start=(c == 0), stop=True),
                     reads=[('OHB', c), 'ltri_b'], writes=[('ppf', c % 2)])
                S.op('dve', lambda pp=pp: V.tensor_tensor(out=base[:], in0=pp[:, 0:32], in1=pstart[:], op=ALU.add), reads=[('ppf', c % 2), 'pstart'], writes=['base'])
                for k in range(2):
                    S.op('dve', lambda k=k: V.tensor_tensor(out=tmp[:, 0:32], in0=OH12[:, c * 64 + k * 32: c * 64 + (k + 1) * 32], in1=base[:], op=ALU.mult),
                         reads=[('OH12', c), 'base'], writes=['tmp'])
                    S.op('dve', lambda k=k: V.tensor_reduce(out=dstf[:, c * 2 + k: c * 2 + k + 1], in_=tmp[:, 0:32], axis=AX.X, op=ALU.add), reads=['tmp'], writes=['dstf'])
            S.op('dve', lambda: V.tensor_copy(out=DEST[:], in_=dstf[:]), reads=['dstf'], writes=['DEST'])
            for c in range(16):
                b = c % 2
                S.dma('sp', lambda c=c, b=b: nc.sync.dma_start(out=hbx[b][:], in_=h1_d[c * 128:(c + 1) * 128, :]), 'xin%d' % b, reads=[('h1_d', c)], writes=[('hbx', b)])
                S.op('act', lambda b=b: A.copy(out=hbb[b][:], in_=hbx[b][:]), reads=[('hbx', b)], writes=[('hbb', b)])
                for k in range(2):
                    S.dma('pool', lambda c=c, b=b, k=k: G.indirect_dma_start(
                        out=xs_d[:, :], out_offset=bass.IndirectOffsetOnAxis(ap=DEST[:, c * 2 + k: c * 2 + k + 1], axis=0), in_=hbb[b][:], in_offset=None,
                        bounds_check=R_XS, oob_is_err=False), 'sct%d' % b, reads=[('hbb', b), 'DEST'], writes=[('xs_d', c, k)])
            S.barrier()
            if stop == 4:
                S.dma('sp', lambda: nc.sync.dma_start(out=dbg[:, 0:32], in_=GATE[:]), 'dbg')
                S.dma('sp', lambda: nc.sync.dma_start(out=dbg[:, 32:64], in_=dstf[:]), 'dbg')
                S.dma('sp', lambda: nc.sync.dma_start(out=dbg[:, 64:128], in_=be[:]), 'dbg')
                S.dma('sp', lambda: nc.sync.dma_start(out=dbg[:, 128:160], in_=pend[:]), 'dbg')
                S.dma('sp', lambda: nc.sync.dma_start(out=dbg[:, 1024:2048], in_=OH12[:]), 'dbg')
                S.muted = True

        with contextlib.ExitStack() as e7:
            SC("s7")
            NS13, NS2 = 16, 8
            W13r = sb(e7, "W13r", [128, NS13 * 2048], BF16)
            W2r = sb(e7, "W2r", [128, NS2 * 2048], BF16)
            xblk = [sb(e7, "xblk%d" % i, [128, D], BF16) for i in range(2)]
            xT = [sb(e7, "xT%d" % i, [128, 16 * 128], BF16) for i in range(2)]
            sg = sb(e7, "sg", [128, 1024], F32)
            act2 = [sb(e7, "act%d" % i, [128, 1024], BF16) for i in range(2)]
            actT = sb(e7, "actT", [128, 8 * 128], BF16)
            yb = [sb(e7, "yb%d" % i, [128, D], F32) for i in range(2)]
            ptx = [pt(e7, "ptx%d" % i, [128, 1024], BF16) for i in range(2)]
            ph = [pt(e7, "ph%d" % i, [128, 512], F32) for i in range(4)]
            py = [pt(e7, "py%d" % i, [128, 512], F32) for i in range(2)]

            def gather13(n):
                s_ = n % NS13
                S.dma('pool', lambda: G.indirect_dma_start(out=W13r[:, s_ * 2048:(s_ + 1) * 2048], out_offset=None, in_=w13[:, :],
                                                           in_offset=bass.IndirectOffsetOnAxis(ap=IDX13[:, n:n + 1], axis=0),
                                                           bounds_check=R_W13, oob_is_err=False), 'g13_%d' % s_, reads=['IDX13'], writes=[('W13', s_)])

            def gather2(n):
                s_ = n % NS2
                S.dma('pool', lambda: G.indirect_dma_start(out=W2r[:, s_ * 2048:(s_ + 1) * 2048], out_offset=None, in_=w2[:, :],
                                                           in_offset=bass.IndirectOffsetOnAxis(ap=IDX2[:, n:n + 1], axis=0),
                                                           bounds_check=R_W2, oob_is_err=False), 'g2_%d' % s_, reads=['IDX2'], writes=[('W2', s_)])

            def xload(j):
                b = j % 2
                S.dma('sp', lambda: nc.sync.dma_start(out=xblk[b][:], in_=xs_d[j * 128:(j + 1) * 128, :]), 'xin%d' % b, reads=[], writes=[('xblk', b)])

            def xtr(j):
                b = j % 2
                for hf in (1, 0):
                    for k8 in range(8):
                        dc = hf * 8 + k8
                        S.op('pe', lambda dc=dc, k8=k8: PE.transpose(ptx[hf][:, k8 * 128:(k8 + 1) * 128], xblk[b][:, dc * 128:(dc + 1) * 128], ident_b[:]),
                             reads=[('xblk', b), 'ident_b'], writes=[('ptx', hf)])
                    S.op('act', lambda hf=hf: A.copy(out=xT[b][:, hf * 1024:(hf + 1) * 1024], in_=ptx[hf][:]), reads=[('ptx', hf)], writes=[('xT', b, hf)])

            def mm1(j):
                b = j % 2
                for dc in range(16):
                    n = j * 16 + dc
                    s_ = n % NS13
                    for fb in range(4):
                        S.op('pe', lambda dc=dc, fb=fb, s_=s_: PE.matmul(ph[fb][:], lhsT=xT[b][:, dc * 128:(dc + 1) * 128],
                                                                         rhs=W13r[:, s_ * 2048 + fb * 512: s_ * 2048 + (fb + 1) * 512], start=(dc == 0), stop=(dc == 15)),
                             reads=[('xT', b, dc // 8), ('W13', s_)], writes=[('ph', fb)])
                    if n + NS13 < NBLK * 16:
                        gather13(n + NS13)

            def actf(j):
                for hf in range(2):
                    S.op('act', lambda hf=hf: A.activation(out=sg[:, hf * 512:(hf + 1) * 512], in_=ph[hf][:], func=AF.Silu), reads=[('ph', hf)], writes=[('sg', hf)])
                    S.op('dve', lambda hf=hf: V.tensor_tensor(out=act2[j % 2][:, hf * 512:(hf + 1) * 512], in0=sg[:, hf * 512:(hf + 1) * 512], in1=ph[2 + hf][:], op=ALU.mult),
                         reads=[('sg', hf), ('ph', 2 + hf)], writes=[('act', j % 2, hf)])

            def atr(j):
                for fc in range(8):
                    S.op('pe', lambda fc=fc: PE.transpose(ptx[0][:, fc * 128:(fc + 1) * 128], act2[j % 2][:, fc * 128:(fc + 1) * 128], ident_b[:]),
                         reads=[('act', j % 2, fc // 4), 'ident_b'], writes=[('ptx', 0)])
                S.op('act', lambda: A.copy(out=actT[:], in_=ptx[0][:]), reads=[('ptx', 0)], writes=['actT'])

            def mm2(j):
                b = j % 2
                for hf in range(2):
                    for fc in range(8):
                        n = j * 8 + fc
                        s_ = n % NS2
                        for fb in range(2):
                            S.op('pe', lambda fc=fc, fb=fb, s_=s_, hf=hf: PE.matmul(py[fb][:], lhsT=actT[:, fc * 128:(fc + 1) * 128],
                                                                                    rhs=W2r[:, s_ * 2048 + (hf * 2 + fb) * 512: s_ * 2048 + (hf * 2 + fb + 1) * 512],
                                                                                    start=(fc == 0), stop=(fc == 7)),
                                 reads=['actT', ('W2', s_)], writes=[('py', fb)])
                        if hf == 1 and n + NS2 < NBLK * 8:
                            gather2(n + NS2)
                    S.op('act', lambda hf=hf: A.copy(out=yb[b][:, hf * 1024: hf * 1024 + 512], in_=py[0][:]), reads=[('py', 0)], writes=[('yb', b, hf)])
                    S.op('dve', lambda hf=hf: V.tensor_copy(out=yb[b][:, hf * 1024 + 512: hf * 1024 + 1024], in_=py[1][:]), reads=[('py', 1)], writes=[('yb', b, hf)])
                S.dma('sp', lambda: nc.sync.dma_start(out=ys_d[j * 128:(j + 1) * 128, :], in_=yb[b][:]), 'yst%d' % b, reads=[('yb', b, 0), ('yb', b, 1)], writes=[('ys_d', j)])

            for n in range(NS13):
                gather13(n)
            for n in range(NS2):
                gather2(n)
            xload(0)
            xload(1)
            xtr(0)
            for j in range(NBLK):
                mm1(j)
                actf(j)
                if j >= 1:
                    atr(j - 1)
                if j + 1 < NBLK:
                    xtr(j + 1)
                if j + 2 < NBLK:
                    xload(j + 2)
                if j >= 1:
                    mm2(j - 1)
            atr(NBLK - 1)
            mm2(NBLK - 1)
            S.barrier()
            if stop == 7:
                S.muted = True

        with contextlib.ExitStack() as e8:
            SC("s8")
            gb2 = sb(e8, "gb2", [128, 2 * D], F32)
            hx = [sb(e8, "hx%d" % i, [128, D], F32) for i in range(4)]
            y1 = [sb(e8, "y1_%d" % i, [128, D], F32) for i in range(4)]
            y2 = [sb(e8, "y2_%d" % i, [128, D], F32) for i in range(4)]
            S.dma('sp', lambda: nc.sync.dma_start(out=gb2[:], in_=gb_d[:, 4 * D:6 * D]), 'cst', writes=['gb2'])
            for c in range(16):
                b = c % 4
                x_ = hx[b]
                S.dma('sp', lambda: nc.sync.dma_start(out=x_[:], in_=h1_d[c * 128:(c + 1) * 128, :]), 'hxl%d' % b, reads=[('h1_d', c)], writes=[('hx', b)])
                for k, yt in enumerate((y1[b], y2[b])):
                    S.dma('pool', lambda k=k, yt=yt: G.indirect_dma_start(out=yt[:], out_offset=None, in_=ys_d[:, :],
                                                                          in_offset=bass.IndirectOffsetOnAxis(ap=DEST[:, c * 2 + k: c * 2 + k + 1], axis=0),
                                                                          bounds_check=R_XS, oob_is_err=False), 'yg%d%d' % (k, b),
                          reads=['DEST'], writes=[('y', k, b)])
                S.op('act', lambda: A.activation(out=x_[:], in_=x_[:], func=AF.Identity, scale=ALPHA), reads=[('hx', b)], writes=[('hx', b)])
                for k, yt in enumerate((y1[b], y2[b])):
                    S.op('dve', lambda k=k, yt=yt: V.scalar_tensor_tensor(out=x_[:], in0=yt[:], scalar=GATE[:, c * 2 + k: c * 2 + k + 1], in1=x_[:], op0=ALU.mult, op1=ALU.add),
                         reads=[('y', k, b), ('hx', b), ('GATE', c)], writes=[('hx', b)])
                mean, rstd, rk = ln_stats(x_, ('hx', b), gi)
                gi += 1
                normalize(x_[:], x_[:], mean, rstd, [('hx', b)] + rk, [('hx', b)])
                S.op('pool', lambda: G.tensor_tensor(out=x_[:], in0=x_[:], in1=gb2[:, 0:D], op=ALU.mult), reads=[('hx', b), 'gb2'], writes=[('hx', b)])
                S.op('dve', lambda: V.tensor_tensor(out=x_[:], in0=x_[:], in1=gb2[:, D:2 * D], op=ALU.add), reads=[('hx', b), 'gb2'], writes=[('hx', b)])
                S.dma('sp', lambda: nc.sync.dma_start(out=out[c * 128:(c + 1) * 128, :], in_=x_[:]), 'ost%d' % b, reads=[('hx', b)], writes=[('out', c)])
            S.muted = False
            S.barrier()
            scope.close()
    return nc


_NC = None


def _consts(half):
    ident = np.eye(128, dtype=np.float32)
    tp = np.arange(128)
    ltri = (tp[:, None] < tp[None, :]).astype(np.float32)
    utri = (tp[:, None] <= tp[None, :]).astype(np.float32)
    masks = np.zeros((128, 18, 512), np.float32)
    k = np.arange(128)[:, None]
    q = np.arange(512)[None, :]
    for s_ in range(2):
        off = TILES[half][s_] % 2
        for i in range(9):
            masks[:, s_ * 9 + i, :] = (128 * i + k <= 16 + 512 * off + q)
    return ident, ltri, utri, masks.reshape(128, 18 * 512)


def half_off(half):
    return half


def kernel(x, meta, ln_in_g, ln_in_b, w_in, b_f, pool_w, pool_scale, w_out, ln_mix_g, ln_mix_b,
           w_router_g, b_router_g, w_router_e, b_router_e, w13, w2, ln_ffn_g, ln_ffn_b):
    global _NC
    import os
    stop = float(os.environ.get("KSTOP", "99"))
    ncores = int(os.environ.get("KCORES", "8"))
    f = np.float32
    x = np.asarray(x, f)
    meta = np.asarray(meta, f)
    if _NC is None:
        _NC = build(stop)
    nc = _NC
    w_in0 = np.ascontiguousarray(np.asarray(w_in, f)[0])
    w_out0 = np.ascontiguousarray(np.asarray(w_out, f)[0])
    pool_w0 = np.ascontiguousarray(np.asarray(pool_w, f)[0].reshape(4 * 256, 256))
    w13_0 = np.ascontiguousarray(np.asarray(w13, f)[0].reshape(32 * 2048, 2048))
    w2_0 = np.ascontiguousarray(np.asarray(w2, f)[0].reshape(32 * 1024, 2048))
    gT = np.ascontiguousarray(np.asarray(ln_in_g, f).reshape(16, 128).T)
    bT = np.ascontiguousarray(np.asarray(ln_in_b, f).reshape(16, 128).T)
    rep = lambda v: np.broadcast_to(np.asarray(v, f).reshape(1, -1), (128, np.asarray(v).size))
    gb = np.ascontiguousarray(np.concatenate([rep(ln_in_g), rep(ln_in_b), rep(ln_mix_g[0]), rep(ln_mix_b[0]),
                                              rep(ln_ffn_g[0]), rep(ln_ffn_b[0])], axis=1))
    pscale = np.ascontiguousarray(np.asarray(pool_scale, f)[0].reshape(8, 128).T)
    bf_b = np.ascontiguousarray(rep(b_f[0]))
    brt_b = np.ascontiguousarray(np.concatenate([rep(b_router_g[0]), rep(b_router_e[0])], axis=1))
    wr_full = np.concatenate([np.asarray(w_router_g, f)[0], np.asarray(w_router_e, f)[0]], axis=1)
    wr = np.ascontiguousarray(wr_full.reshape(16, 128, 36).transpose(1, 0, 2).reshape(128, 16 * 36))
    iota = np.ascontiguousarray((np.arange(128, dtype=f)[:, None] + 128.0 * np.arange(24, dtype=f)[None, :]))
    jv = np.ascontiguousarray(np.broadcast_to((128.0 * np.arange(NBLK, dtype=f))[None, :, None], (128, NBLK, 32)).reshape(128, NBLK * 32))
    in_maps = []
    for c in range(8):
        b, half = c // 2, c % 2
        xfull = np.zeros((LP, D), f)
        xfull[0:16] = meta
        xfull[16:16 + 4096] = x[b]
        xown = np.zeros((4, 640, D), f)
        sel = np.zeros((LP, 16), f)
        for p, T in enumerate(TILES[half]):
            ps = 16 + 512 * T
            lo = ps - 128
            if lo < 0:
                xown[p, -lo:] = xfull[0:ps + 512]
            else:
                xown[p] = xfull[lo:ps + 512]
            for j in range(4):
                sel[ps + 128 * j + 127, 4 * p + j] = 1.0
        sel_l = np.ascontiguousarray(sel.reshape(NCH, 128, 16).transpose(1, 0, 2).reshape(128, NCH * 16))
        ident, ltri, utri, masks = _consts(half)
        in_maps.append(dict(xfull=xfull, xown=xown.reshape(4 * 640, D), w_in=w_in0, w_out=w_out0, pool_w=pool_w0, w13=w13_0, w2=w2_0,
                            gT=gT, bT=bT, gb=gb, pscale=pscale, bf_b=bf_b, brt_b=brt_b, wr=wr, ident=ident, ltri=ltri, utri=utri,
                            masks=masks, sel=sel_l, iota=iota, jv=jv))
    if stop < 7:
        for m in in_maps:
            m["w13"] = w13_0[:128]
            m["w2"] = w2_0[:128]
    if os.environ.get("KTRACE"):
        res = run_bass_kernel_spmd(nc, in_maps[:ncores], core_ids=list(range(ncores)), trace=True)
        print("EXEC_NS", res.exec_time_ns, "mean", res.mean_exec_time_ns, "maxcore", res.max_exec_time_core_id)
        for k_, v_ in (res.per_core_scope_times or {}).items():
            print("SCOPE", k_, {c_: round(t_ / 1000) for c_, t_ in v_.items()})
    else:
        res = run_bass_kernel_spmd(nc, in_maps[:ncores], core_ids=list(range(ncores)))
    if stop < 99:
        global _DBG
        _DBG = (res, in_maps)
        return None
    outp = np.zeros((4, 4096, D), f)
    for c in range(8):
        b, half = c // 2, c % 2
        o = res.results[c]["out"]
        for p, T in enumerate(TILES[half]):
            outp[b, T * 512:(T + 1) * 512] = o[p * 512:(p + 1) * 512]
    return outp
```
